# Optimizing a Trainium2 kernel written in Bass

```python
import jax, jax.numpy as jnp
from jax import lax
import numpy as np

D_MODEL = 4096
BATCH = 4
SEQ = 2048
DEPTH = 2

D_MIX = D_MODEL
N_MIXERS = 4
GROUP_W = D_MIX // N_MIXERS
HEAD_DIM = 128
N_HEADS = GROUP_W // HEAD_DIM
CHUNK = 128
CONV_W = 4
LRU_C = 8.0
ROPE_BASE = 10000.0
N_EXPERTS = 64
TOP_K = 8
D_EXPERT = D_MODEL // 16
ROUTED_SCALE = 2.5
N_MOD = 6
EPS = 1e-6
N_IN_BLOCKS = 11
N_IN = N_IN_BLOCKS * GROUP_W + N_HEADS

kernel_name = "hybrid_parallel_heads_moe_adaln"


def rms_norm(x, g):
    xf = x.astype(jnp.float32)
    y = xf * lax.rsqrt(jnp.mean(xf * xf, axis=-1, keepdims=True) + EPS)
    return (y * g).astype(x.dtype)


def rms_normalize(x):
    xf = x.astype(jnp.float32)
    return (xf * lax.rsqrt(jnp.mean(xf * xf, axis=-1, keepdims=True) + EPS)).astype(x.dtype)


def heads(t):
    return t.reshape(t.shape[:-1] + (N_HEADS, HEAD_DIM))


def rotary(t, pos):
    half = HEAD_DIM // 2
    inv = ROPE_BASE ** (-jnp.arange(half, dtype=jnp.float32) / half)
    ang = pos[:, None] * inv[None, :]
    cos = jnp.cos(ang)[:, None, :].astype(t.dtype)
    sin = jnp.sin(ang)[:, None, :].astype(t.dtype)
    t1, t2 = t[..., :half], t[..., half:]
    return jnp.concatenate([t1 * cos - t2 * sin, t2 * cos + t1 * sin], axis=-1)


def retention(q, k, v, g):
    B, S, _ = q.shape
    n = S // CHUNK
    pos = jnp.arange(S, dtype=jnp.float32)
    q = rotary(heads(q), pos)
    k = rotary(heads(k), pos) * (HEAD_DIM ** -0.5)
    v = heads(v)
    chunks = lambda t: t.reshape(B, n, CHUNK, N_HEADS, HEAD_DIM).transpose(0, 3, 1, 2, 4)
    qc, kc, vc = chunks(q), chunks(k), chunks(v)
    log_gamma = jnp.log1p(-2.0 ** (-5.0 - jnp.arange(N_HEADS, dtype=jnp.float32)))
    idx = jnp.arange(CHUNK, dtype=jnp.float32)
    rel = idx[:, None] - idx[None, :]
    decay_intra = jnp.exp(jnp.where(rel >= 0, rel[None] * log_gamma[:, None, None], -jnp.inf))
    scores = jnp.einsum('bhncd,bhnmd->bhncm', qc, kc) * decay_intra[:, None]
    intra = jnp.einsum('bhncm,bhnme->bhnce', scores, vc)
    k_decay = jnp.exp((CHUNK - 1 - idx)[None, :] * log_gamma[:, None])
    q_decay = jnp.exp((idx + 1)[None, :] * log_gamma[:, None])
    chunk_kv = jnp.einsum('bhnmd,bhnme->nbhde', kc * k_decay[:, None, :, None], vc)
    chunk_decay = jnp.exp(CHUNK * log_gamma)[:, None, None]

    def step(state, kv):
        return chunk_decay * state + kv, state

    _, prev = lax.scan(step, jnp.zeros_like(chunk_kv[0]), chunk_kv)
    inter = jnp.einsum('bhncd,nbhde->bhnce', qc * q_decay[:, None, :, None], prev)
    y = (intra + inter).astype(q.dtype).transpose(0, 2, 3, 1, 4).reshape(B, S, N_HEADS, HEAD_DIM)
    y = rms_normalize(y).reshape(B, S, GROUP_W)
    return jax.nn.silu(g) * y


def rg_lru_branch(gate_in, x_in, conv_w, conv_b, wa, ba, wx, bx, lam):
    B, S, _ = x_in.shape
    xc = lax.conv_general_dilated(x_in, conv_w[:, None, :], window_strides=(1,),
                                  padding=[(CONV_W - 1, 0)], dimension_numbers=('NWC', 'WIO', 'NWC'),
                                  feature_group_count=GROUP_W) + conv_b
    xg = xc.reshape(B, S, N_HEADS, HEAD_DIM)
    r = jax.nn.sigmoid(jnp.einsum('bsgi,gij->bsgj', xg, wa).reshape(B, S, GROUP_W) + ba)
    i = jax.nn.sigmoid(jnp.einsum('bsgi,gij->bsgj', xg, wx).reshape(B, S, GROUP_W) + bx)
    log_a = (-LRU_C * r * jax.nn.softplus(-lam)).astype(jnp.float32)
    a = jnp.exp(log_a)
    b = jnp.sqrt(-jnp.expm1(2.0 * log_a)) * (i * xc).astype(jnp.float32)

    def combine(left, right):
        a1, b1 = left
        a2, b2 = right
        return a1 * a2, a2 * b1 + b2

    _, h = lax.associative_scan(combine, (a, b), axis=1)
    return h.astype(x_in.dtype) * jax.nn.gelu(gate_in)


def chunked_sgu(u, v, norm_g, w_s, b_s):
    B, S, _ = u.shape
    n = S // CHUNK
    u = jax.nn.gelu(u)
    v = rms_norm(jax.nn.gelu(v), norm_g)
    vc = v.reshape(B, n, CHUNK, N_HEADS, HEAD_DIM)
    mask = jnp.tril(jnp.ones((CHUNK, CHUNK), dtype=bool))
    w = jnp.where(mask[None], w_s, 0.0)
    mixed = jnp.einsum('gts,bnsgc->bntgc', w, vc) + b_s.T[:, :, None]
    return u * mixed.reshape(B, S, GROUP_W)


def forgetting_attention(q, k, v, f_logit, qn, kn, fb):
    B, S, _ = q.shape
    n = S // CHUNK
    q = rms_norm(heads(q), qn).transpose(0, 2, 1, 3)
    k = rms_norm(heads(k), kn).transpose(0, 2, 1, 3)
    v = heads(v).transpose(0, 2, 1, 3)
    log_f = jax.nn.log_sigmoid((f_logit + fb).astype(jnp.float32))
    cum = jnp.cumsum(log_f, axis=1).transpose(0, 2, 1)
    scale = HEAD_DIM ** -0.5
    outs = []
    for blk in range(n):
        q0, q1 = blk * CHUNK, (blk + 1) * CHUNK
        logits = jnp.einsum('bhqd,bhkd->bhqk', q[:, :, q0:q1], k[:, :, :q1]).astype(jnp.float32) * scale
        logits = logits + cum[:, :, q0:q1, None] - cum[:, :, None, :q1]
        causal = (q0 + jnp.arange(CHUNK))[:, None] >= jnp.arange(q1)[None, :]
        p = jax.nn.softmax(jnp.where(causal, logits, -jnp.inf), axis=-1).astype(v.dtype)
        outs.append(jnp.einsum('bhqk,bhkd->bhqd', p, v[:, :, :q1]))
    o = jnp.concatenate(outs, axis=2)
    return o.transpose(0, 2, 1, 3).reshape(B, S, GROUP_W)


def hybrid_mixer(h, w_in, w_out, conv_w, conv_b, wa, ba, wx, bx, lam, sgu_g, sgu_w, sgu_b, qn, kn, fb):
    proj = h @ w_in
    (rq, rk, rv, rg, lg, lx, su, sv, fq, fk, fv, ff) = jnp.split(
        proj, [GROUP_W * i for i in range(1, N_IN_BLOCKS + 1)], axis=-1)
    y = jnp.concatenate([
        retention(rq, rk, rv, rg),
        rg_lru_branch(lg, lx, conv_w, conv_b, wa, ba, wx, bx, lam),
        chunked_sgu(su, sv, sgu_g, sgu_w, sgu_b),
        forgetting_attention(fq, fk, fv, ff, qn, kn, fb),
    ], axis=-1)
    return y @ w_out


def moe(h, router_w, router_bias, w_gate, w_up, w_down, sw_gate, sw_up, sw_down):
    B, S, D = h.shape
    t = h.reshape(B * S, D)
    scores = jax.nn.sigmoid((t @ router_w).astype(jnp.float32))
    _, idx = lax.top_k(scores + router_bias, TOP_K)
    sel = jnp.take_along_axis(scores, idx, axis=-1)
    wts = sel / jnp.sum(sel, axis=-1, keepdims=True) * ROUTED_SCALE
    gates = jnp.sum(jax.nn.one_hot(idx, N_EXPERTS, dtype=wts.dtype) * wts[..., None], axis=1)
    hidden = jax.nn.silu(jnp.einsum('td,edf->tef', t, w_gate)) * jnp.einsum('td,edf->tef', t, w_up)
    routed = jnp.einsum('tef,efd->td', hidden * gates[:, :, None].astype(hidden.dtype), w_down)
    shared = (jax.nn.silu(t @ sw_gate) * (t @ sw_up)) @ sw_down
    return (routed + shared).reshape(B, S, D)


def setup_inputs(seed: int = 0) -> dict:
    key = jax.random.key(seed)
    ks = jax.random.split(key, 32)
    nrm = lambda k, shape, s: jax.random.normal(k, shape, jnp.float32) * s
    L, D, G, H, Dh, E, F = DEPTH, D_MODEL, GROUP_W, N_HEADS, HEAD_DIM, N_EXPERTS, D_EXPERT
    u = jax.random.uniform(ks[15], (L, G), jnp.float32, minval=0.9, maxval=0.999)
    a0 = u ** (1.0 / LRU_C)
    return {
        'x': nrm(ks[0], (BATCH, SEQ, D), 1.0),
        'c': nrm(ks[1], (BATCH, D), 1.0),
        'w_ada': nrm(ks[2], (D, N_MOD * D), 0.1 * D ** -0.5),
        'b_ada': nrm(ks[3], (N_MOD * D,), 0.01),
        'ada_table': nrm(ks[4], (L, N_MOD, D), 0.2),
        'norm1_g': 1.0 + nrm(ks[5], (L, D), 0.02),
        'norm2_g': 1.0 + nrm(ks[6], (L, D), 0.02),
        'w_in': nrm(ks[7], (L, D, N_IN), D ** -0.5),
        'w_out': nrm(ks[8], (L, D_MIX, D), D_MIX ** -0.5),
        'lru_conv_w': nrm(ks[9], (L, CONV_W, G), CONV_W ** -0.5),
        'lru_conv_b': nrm(ks[10], (L, G), 0.01),
        'lru_wa': nrm(ks[11], (L, H, Dh, Dh), Dh ** -0.5),
        'lru_ba': nrm(ks[12], (L, G), 0.01),
        'lru_wx': nrm(ks[13], (L, H, Dh, Dh), Dh ** -0.5),
        'lru_bx': nrm(ks[14], (L, G), 0.01),
        'lru_lambda': jnp.log(a0) - jnp.log1p(-a0),
        'sgu_norm_g': 1.0 + nrm(ks[16], (L, G), 0.02),
        'sgu_w': nrm(ks[17], (L, H, CHUNK, CHUNK), 0.5 * CHUNK ** -0.5),
        'sgu_b': 1.0 + nrm(ks[18], (L, H, CHUNK), 0.02),
        'fox_qn': 1.0 + nrm(ks[19], (L, Dh), 0.02),
        'fox_kn': 1.0 + nrm(ks[20], (L, Dh), 0.02),
        'fox_fb': 3.0 + nrm(ks[21], (L, H), 0.5),
        'router_w': nrm(ks[22], (L, D, E), D ** -0.5),
        'router_bias': nrm(ks[23], (L, E), 0.01),
        'exp_w_gate': nrm(ks[24], (L, E, D, F), D ** -0.5),
        'exp_w_up': nrm(ks[25], (L, E, D, F), D ** -0.5),
        'exp_w_down': nrm(ks[26], (L, E, F, D), F ** -0.5),
        'sh_w_gate': nrm(ks[27], (L, D, F), D ** -0.5),
        'sh_w_up': nrm(ks[28], (L, D, F), D ** -0.5),
        'sh_w_down': nrm(ks[29], (L, F, D), F ** -0.5),
    }


def reference(x, c, w_ada, b_ada, ada_table, norm1_g, norm2_g, w_in, w_out,
              lru_conv_w, lru_conv_b, lru_wa, lru_ba, lru_wx, lru_bx, lru_lambda,
              sgu_norm_g, sgu_w, sgu_b, fox_qn, fox_kn, fox_fb,
              router_w, router_bias, exp_w_gate, exp_w_up, exp_w_down,
              sh_w_gate, sh_w_up, sh_w_down):
    B = x.shape[0]
    mod_shared = (jax.nn.silu(c) @ w_ada + b_ada).reshape(B, N_MOD, D_MODEL)
    for l in range(DEPTH):
        mod = mod_shared + ada_table[l][None]
        shift1, scale1, gate1, shift2, scale2, gate2 = [mod[:, i][:, None, :] for i in range(N_MOD)]
        h = rms_norm(x, norm1_g[l]) * (1.0 + scale1) + shift1
        mix = hybrid_mixer(h, w_in[l], w_out[l], lru_conv_w[l], lru_conv_b[l], lru_wa[l], lru_ba[l],
                           lru_wx[l], lru_bx[l], lru_lambda[l], sgu_norm_g[l], sgu_w[l], sgu_b[l],
                           fox_qn[l], fox_kn[l], fox_fb[l])
        x = x + gate1 * mix
        h = rms_norm(x, norm2_g[l]) * (1.0 + scale2) + shift2
        x = x + gate2 * moe(h, router_w[l], router_bias[l], exp_w_gate[l], exp_w_up[l], exp_w_down[l],
                            sh_w_gate[l], sh_w_up[l], sh_w_down[l])
    return x
```

```python
import numpy as np
from contextlib import ExitStack
import concourse.bass as bass
import concourse.mybir as mybir
from concourse.bass_utils import run_bass_kernel_spmd

F32 = mybir.dt.float32
BF16 = mybir.dt.bfloat16
AF = mybir.ActivationFunctionType
ALU = mybir.AluOpType
AX = mybir.AxisListType

NR = 8
HD = 128


class Cfg:
    def __init__(self, D=4096, S=2048, B=4, E=64, L=2, F=256, NCORE=4):
        self.D, self.S, self.B, self.E, self.L, self.F, self.NCORE = D, S, B, E, L, F, NCORE
        self.KC = D // 128
        self.BL = B // NCORE
        self.NTL = self.BL * S
        self.NCH = S // 128
        self.NG = E // 8
        assert S % 512 == 0 and E % 8 == 0 and B % NCORE == 0


class Buf:
    __slots__ = ("name", "writer", "readers", "sem")

    def __init__(self, name):
        self.name = name
        self.writer = None
        self.readers = {}
        self.sem = None


class SemSlot:
    def __init__(self, sem):
        self.sem = sem
        self.count = 0
        self.bg = False


class Prog:
    ENGS = ("pe", "act", "dve", "pool", "sp")

    def __init__(self, nc, es):
        self.nc, self.es = nc, es
        self.q = {e: [] for e in self.ENGS}
        self.esem = {e: es.enter_context(nc.semaphore("S_" + e)) for e in self.ENGS}
        self.allbufs = []
        self.active = []
        self.free = []
        self.nslots = 0
        self.bgbufs = set()

    def buf(self, name=None):
        b = Buf(f"{name or 'b'}{len(self.allbufs)}")
        self.allbufs.append(b)
        return b

    def bufs(self, n, name="b"):
        return [self.buf(name) for _ in range(n)]

    def op(self, eng, fn, reads=(), writes=()):
        deps = []
        for b in reads:
            if b.writer is not None:
                deps.append(b.writer)
        for b in writes:
            w = b.writer
            if w is not None and not (w[0] == 'op' and w[1] == eng):
                deps.append(w)
            for r in b.readers.values():
                if not (r[0] == 'op' and r[1] == eng):
                    deps.append(r)
        if eng == "pe":
            deps = [d for d in deps if not (d[0] == 'op' and d[1] == 'pe')]
        idx = len(self.q[eng])
        self.q[eng].append({"fn": fn, "deps": deps, "needed": False, "dma": None})
        me = ('op', eng, idx)
        for b in reads:
            b.readers[eng] = me
        for b in writes:
            b.writer = me
            b.readers = {}
        return me

    def dma(self, eng, out, in_, reads=(), writes=(), sem_buf=None, slow=False, bg=False):
        sb = sem_buf or (writes[0] if writes else reads[0])
        if sb.sem is None:
            if self.free and not bg:
                sb.sem = self.free.pop()
            else:
                self.nslots += 1
                sb.sem = SemSlot(self.es.enter_context(self.nc.semaphore(f"D{self.nslots}")))
            if bg:
                sb.sem.bg = True
                self.bgbufs.add(id(sb))
            self.active.append(sb)
        slot = sb.sem
        deps = []
        for b in reads:
            if b.writer is not None:
                deps.append(b.writer)
        for b in writes:
            if b.writer is not None:
                deps.append(b.writer)
            deps.extend(b.readers.values())
        slot.count += 1
        me = ('dma', slot, slot.count)
        if slow:
            fn = (lambda e, o=out, i=in_: e.dma_start(out=o, in_=i, allow_slow_non_contiguous=True))
        else:
            fn = (lambda e, o=out, i=in_: e.dma_start(out=o, in_=i))
        self.q[eng].append({"fn": fn, "deps": deps, "needed": False, "dma": slot})
        for b in reads:
            b.readers[('dma', id(slot))] = me
        for b in writes:
            b.writer = me
            b.readers = {}
        return me

    def barrier(self, final=False):
        deps = []
        for e in self.ENGS:
            for i in range(len(self.q[e]) - 1, -1, -1):
                it = self.q[e][i]
                if it["dma"] is None and it["fn"] is not None:
                    deps.append(('op', e, i))
                    break
        for b in self.active:
            if final or not b.sem.bg:
                deps.append(('dma', b.sem, b.sem.count))
        for e in self.ENGS:
            self.q[e].append({"fn": None, "deps": list(deps), "needed": False, "dma": None})
        keep = []
        for b in self.active:
            if b.sem.bg:
                keep.append(b)
            else:
                self.free.append(b.sem)
                b.sem = None
        self.active = keep
        for b in self.allbufs:
            if id(b) in self.bgbufs:
                continue
            b.writer = None
            b.readers = {}

    def emit(self):
        nc = self.nc
        for e in self.ENGS:
            for it in self.q[e]:
                for d in it["deps"]:
                    if d[0] == 'op':
                        self.q[d[1]][d[2]]["needed"] = True
        cnt = {}
        for e in self.ENGS:
            c = 0
            arr = []
            for it in self.q[e]:
                if it["needed"]:
                    c += 1
                arr.append(c)
            cnt[e] = arr
        self.stats = {e: (len(self.q[e]), cnt[e][-1] if cnt[e] else 0) for e in self.ENGS}
        self.stats["slots"] = self.nslots

        def replay(ename, eng):
            waited = {}
            for i, it in enumerate(self.q[ename]):
                for d in it["deps"]:
                    if d[0] == 'op':
                        if d[1] == ename and d[2] >= i:
                            continue
                        sem = self.esem[d[1]]
                        val = cnt[d[1]][d[2]]
                    else:
                        sem = d[1].sem
                        val = 16 * d[2]
                    key = id(sem)
                    if waited.get(key, 0) >= val:
                        continue
                    waited[key] = val
                    eng.wait_ge(sem, val)
                if it["fn"] is None:
                    continue
                ins = it["fn"](eng)
                if it["dma"] is not None:
                    ins.then_inc(it["dma"].sem, 16)
                elif it["needed"]:
                    ins.then_inc(self.esem[ename], 1)

        with nc.Block() as block:
            @block.tensor
            def _(e):
                replay("pe", e)

            @block.scalar
            def _(e):
                replay("act", e)

            @block.vector
            def _(e):
                replay("dve", e)

            @block.gpsimd
            def _(e):
                replay("pool", e)

            @block.sync
            def _(e):
                replay("sp", e)


class Arena:
    def __init__(self, nc, es, words, name="arena"):
        self.t = es.enter_context(nc.sbuf_tensor(name, [128, words], F32))
        self.words = words
        self.off = 0

    def mark(self):
        return self.off

    def release(self, m):
        self.off = m

    def alloc(self, shape, dtype):
        n = int(np.prod(shape[1:]))
        w = n if dtype == F32 else (n + 1) // 2
        w = (w + 7) // 8 * 8
        assert self.off + w <= self.words, f"arena overflow {self.off}+{w}>{self.words}"
        ap = self.t[0:shape[0], self.off:self.off + w]
        self.off += w
        if dtype != F32:
            ap = ap.bitcast(dtype)
        ap = ap[:, 0:n]
        if len(shape) > 2:
            names = " ".join(f"d{i}" for i in range(1, len(shape)))
            kw = {f"d{i}": shape[i] for i in range(2, len(shape))}
            ap = ap.rearrange(f"p ({names}) -> p {names}", **kw)
        return ap


def build(cfg):
    D, S, B, E, L, KC, BL, NTL, NCH, NG = (cfg.D, cfg.S, cfg.B, cfg.E, cfg.L, cfg.KC, cfg.BL, cfg.NTL,
                                            cfg.NCH, cfg.NG)
    NBK = NTL // 512
    BPB = S // 512
    NCL = BL * NCH
    nc = bass.Bass("TRN2", target_bir_lowering=False)

    def din(name, shape, dt=F32):
        return nc.dram_tensor(name, list(shape), dt, kind="ExternalInput").ap()

    def dint(name, shape, dt=F32):
        return nc.dram_tensor(name, list(shape), dt)

    I = {}
    I["xT"] = din("xT", [D, NTL])
    I["cT"] = din("cT", [128, KC * B])
    I["selb"] = din("selb", [128, BL * B])
    I["wada"] = din("wada", [D, 6 * D])
    I["bada"] = din("bada", [128, 6 * KC])
    I["tab"] = din("tab", [128, L * 6 * KC])
    I["ng"] = din("ng", [128, L * 2 * KC])
    I["win"] = din("win", [L, 8, D, 1409])
    I["wout"] = din("wout", [L, 4096, D])
    I["lrup"] = din("lrup", [128, L * 8 * 8])
    I["lruw"] = din("lruw", [L, 8, 2, 128, 128])
    I["sgbc"] = din("sgbc", [128, L * 8 * 2 * 128])
    I["sgwT"] = din("sgwT", [L, 8, 128, 128])
    I["foxp"] = din("foxp", [128, L * 8 * 3])
    I["rw"] = din("rw", [L, D, E])
    I["rbbc"] = din("rbbc", [128, L * E])
    I["ewg"] = din("ewg", [L, E, 2, 128, KC * 128])
    I["ewu"] = din("ewu", [L, E, 2, 128, KC * 128])
    I["ewd"] = din("ewd", [L, E, 256, D])
    I["swg"] = din("swg", [L, D, 256])
    I["swu"] = din("swu", [L, D, 256])
    I["swd"] = din("swd", [L, 256, D])
    I["selB"] = din("selB", [NG, E, 8 * 128])
    I["cst"] = din("cst", [128, 4 * 128])
    I["hcst"] = din("hcst", [8, 128, 2 * 128 + 2])
    I["rot"] = din("rot", [128, 2 * S])
    outT = nc.dram_tensor("outT", [D, NTL], F32, kind="ExternalOutput").ap()

    hT = dint("hT", [D, NTL], BF16)
    PF = dint("PF", [8, 8 * 128, NTL], F32)
    PT = dint("PT", [8, NTL, 384], F32)
    PTf = dint("PTf", [8, 128, NTL // 128], F32)
    yT = dint("yT", [4096, NTL], BF16)
    xres = dint("xres", [D, NTL], F32)
    shr = dint("shr", [D, NTL], F32)
    gTd = dint("gTd", [E, NTL], F32)
    wgb = dint("wgb", [E, 2, 128, KC * 128], BF16)
    wub = dint("wub", [E, 2, 128, KC * 128], BF16)
    DG = D // 256
    wdb = dint("wdb", [NG, DG, 128, 16 * 256], BF16)

    es = ExitStack()
    P = Prog(nc, es)
    A = Arena(nc, es, 52224)
    ps = [es.enter_context(nc.psum_tensor(f"ps{i}", [128, 512], F32)) for i in range(8)]
    psb = P.bufs(8, "ps")
    psi = [0]

    psmode = [6]

    def nextps():
        k = psi[0] % psmode[0]
        psi[0] += 1
        return ps[k], psb[k]

    def accps(i):
        return ps[psmode[0] + i], psb[psmode[0] + i]

    def run_gens(gens):
        gens = list(gens)
        while gens:
            for g_ in list(gens):
                try:
                    next(g_)
                except StopIteration:
                    gens.remove(g_)

    db = {n: P.buf(n) for n in ["hT", "PF", "PT", "PTf", "yT", "xres", "shr", "gTd", "wgb", "wub", "wdb", "outT"]}
    NB = P.buf("never")

    def mm(out, lhsT, rhs, start, stop, reads, writes):
        return P.op("pe", lambda e: e.matmul(out, lhsT, rhs, start=start, stop=stop), reads, writes)

    def tr(out, in_, ident, reads, writes):
        return P.op("pe", lambda e: e.transpose(out, in_, ident), reads, writes)

    def act(out, in_, func, reads, writes, bias=None, scale=None, accum=None):
        kw = {}
        if bias is not None:
            kw["bias"] = bias
        if scale is not None:
            kw["scale"] = scale
        if accum is not None:
            kw["accum_out"] = accum
        return P.op("act", lambda e: e.activation(out=out, in_=in_, func=func, **kw), reads, writes)

    def tt(eng, out, in0, in1, op, reads, writes):
        return P.op(eng, lambda e: e.tensor_tensor(out=out, in0=in0, in1=in1, op=op), reads, writes)

    def ts(eng, out, in0, s1, s2, op0, op1, reads, writes):
        if op1 is None:
            return P.op(eng, lambda e: e.tensor_scalar(out=out, in0=in0, scalar1=s1, scalar2=None, op0=op0),
                        reads, writes)
        return P.op(eng, lambda e: e.tensor_scalar(out=out, in0=in0, scalar1=s1, scalar2=s2, op0=op0, op1=op1),
                    reads, writes)

    def stt(out, in0, scalar, in1, op0, op1, reads, writes):
        return P.op("dve", lambda e: e.scalar_tensor_tensor(out=out, in0=in0, scalar=scalar, in1=in1,
                                                            op0=op0, op1=op1), reads, writes)

    def cp(eng, out, in_, reads, writes):
        if eng == "act":
            return P.op("act", lambda e: e.copy(out=out, in_=in_), reads, writes)
        return P.op(eng, lambda e: e.tensor_copy(out=out, in_=in_), reads, writes)

    def recip(out, in_, reads, writes):
        return P.op("dve", lambda e: e.reciprocal(out=out, in_=in_), reads, writes)

    cst = A.alloc([128, 4 * 128], F32)
    b_cst = P.buf("cst")
    P.dma("sp", cst, I["cst"], writes=[b_cst])
    ident_f = cst[:, 0:128]
    triu_f = cst[:, 128:256]
    ones_f = cst[:, 256:384]
    Pm_f = cst[:, 384:512]
    cstb = A.alloc([128, 3 * 128], BF16)
    b_cstb = P.buf("cstb")
    cp("dve", cstb, cst[:, 0:384], [b_cst], [b_cstb])
    ident_b = cstb[:, 0:128]
    triu_b = cstb[:, 128:256]
    ones_b = cstb[:, 256:384]
    epsv = A.alloc([128, 2], F32)
    b_eps = P.buf("eps")
    P.op("dve", lambda e: e.memset(epsv[:, 0:1], 1e-6), [], [b_eps])
    P.op("dve", lambda e: e.memset(epsv[:, 1:2], 1.0), [], [b_eps])

    def eps_ap(v):
        return epsv[:, 0:1] if v < 0.5 else epsv[:, 1:2]

    modv = A.alloc([128, BL * L * 6, KC], F32)
    b_modv = P.buf("modv")

    def MV(bl, l, i):
        return modv[:, (bl * L + l) * 6 + i, :]
    srs = A.alloc([128, NCL], F32)
    b_srs = P.buf("srs")

    def phase_mod():
        m0 = A.mark()
        ct = A.alloc([128, KC, B], F32)
        cs = A.alloc([128, KC, B], BF16)
        sg = A.alloc([128, KC, B], F32)
        b_ct, b_cs, b_sg = P.buf("ct"), P.buf("cs"), P.buf("sg")
        P.dma("sp", ct, I["cT"].rearrange("p (k b) -> p k b", b=B), writes=[b_ct])
        act(sg, ct, AF.Sigmoid, [b_ct], [b_sg])
        tt("dve", cs, ct, sg, ALU.mult, [b_ct, b_sg], [b_cs])
        wb = [A.alloc([128, KC, 128], BF16) for _ in range(3)]
        b_wb = P.bufs(3, "wada")
        ma = A.alloc([128, 6 * KC, B], F32)
        b_ma = P.buf("ma")
        wsrc = I["wada"].rearrange("(k p) c -> p k c", p=128)
        NJ = 6 * KC
        JB = 512 // B
        for j in range(NJ):
            w = j % 3
            P.dma("pool", wb[w], wsrc[:, :, j * 128:(j + 1) * 128], writes=[b_wb[w]])
            pt, pb = accps((j // JB) % 2)
            jj = j % JB
            for kc in range(KC):
                mm(pt[:, jj * B:(jj + 1) * B], wb[w][:, kc, :], cs[:, kc, :], kc == 0, kc == KC - 1,
                   [b_wb[w], b_cs], [pb])
            if jj == JB - 1 or j == NJ - 1:
                j0 = (j // JB) * JB
                cp("dve", ma[:, j0:j + 1, :].rearrange("p j b -> p (j b)"), pt[:, 0:(j + 1 - j0) * B], [pb], [b_ma])
        selb = A.alloc([128, BL, B], F32)
        bada = A.alloc([128, 6 * KC], F32)
        tab = A.alloc([128, L, 6 * KC], F32)
        ng = A.alloc([128, L, 2, KC], F32)
        b_small = P.buf("small")
        P.dma("sp", selb, I["selb"].rearrange("p (a b) -> p a b", b=B), writes=[b_small])
        P.dma("sp", bada, I["bada"], writes=[b_small])
        P.dma("sp", tab, I["tab"].rearrange("p (l x) -> p l x", l=L), writes=[b_small])
        P.dma("sp", ng, I["ng"].rearrange("p (l t k) -> p l t k", l=L, t=2), writes=[b_small])
        mm_ = A.alloc([128, 6 * KC], F32)
        ml = A.alloc([128, 6, KC], F32)
        b_mm, b_ml = P.buf("modm"), P.buf("ml")
        for bl in range(BL):
            ts("dve", mm_, ma[:, :, 0], selb[:, bl, 0:1], None, ALU.mult, None, [b_ma, b_small], [b_mm])
            for b in range(1, B):
                stt(mm_, ma[:, :, b], selb[:, bl, b:b + 1], mm_, ALU.mult, ALU.add, [b_ma, b_small, b_mm], [b_mm])
            tt("dve", mm_, mm_, bada, ALU.add, [b_mm, b_small], [b_mm])
            for l in range(L):
                tt("dve", ml.rearrange("p a k -> p (a k)"), mm_, tab[:, l, :], ALU.add, [b_mm, b_small], [b_ml])
                for h in range(2):
                    sh, sc, gt = ml[:, 3 * h + 0, :], ml[:, 3 * h + 1, :], ml[:, 3 * h + 2, :]
                    stt(MV(bl, l, 3 * h + 0), sc, 1.0, ng[:, l, h, :], ALU.add, ALU.mult, [b_ml, b_small], [b_modv])
                    cp("dve", MV(bl, l, 3 * h + 1), sh, [b_ml], [b_modv])
                    cp("dve", MV(bl, l, 3 * h + 2), gt, [b_ml], [b_modv])
        P.barrier()
        A.release(m0)

    def phase_norm(l, h, src_ap, src_buf, extra=None):
        m0 = A.mark()
        xb = A.alloc([128, KC, 512], F32)
        hb = A.alloc([128, KC, 512], BF16)
        sq = [A.alloc([128, 512], BF16) for _ in range(3)]
        rs = A.alloc([128, 512], F32)
        rt = A.alloc([128, 512], F32)
        b_x, b_h, b_rs, b_rt = P.buf("x"), P.buf("h"), P.buf("rs"), P.buf("rt")
        b_sq = P.bufs(3, "sq")
        if extra:
            extra("alloc", None)
        srcv = src_ap.rearrange("(k p) t -> p k t", p=128)
        dstv = hT.ap().rearrange("(k p) t -> p k t", p=128)
        for tb in range(NBK):
            bl = tb // BPB
            tsl = slice(tb * 512, (tb + 1) * 512)
            P.dma("sp", xb, srcv[:, :, tsl], reads=[src_buf], writes=[b_x])
            pt, pb = nextps()
            for kc in range(KC):
                i = kc % 3
                act(sq[i], xb[:, kc, :], AF.Square, [b_x], [b_sq[i]])
                mm(pt[:, :], ones_b, sq[i], kc == 0, kc == KC - 1, [b_sq[i], b_cstb], [pb])
            act(rt, pt[:, :], AF.Sqrt, [pb, b_eps], [b_rt], bias=eps_ap(1e-6), scale=1.0 / D)
            recip(rs, rt, [b_rt], [b_rs])
            tt("dve", xb, xb, rs.unsqueeze(1).to_broadcast([128, KC, 512]), ALU.mult, [b_x, b_rs], [b_x])
            for kc in range(KC):
                act(hb[:, kc, :], xb[:, kc, :], AF.Identity, [b_x, b_modv], [b_h],
                    bias=MV(bl, l, 3 * h + 1)[:, kc:kc + 1], scale=MV(bl, l, 3 * h + 0)[:, kc:kc + 1])
            P.dma("sp", dstv[:, :, tsl], hb, reads=[b_h], writes=[db["hT"]])
            if extra:
                extra("post", (tb, bl, hb, b_h))
        P.barrier()
        A.release(m0)

    def phase_inproj_all(l):
        m0 = A.mark()
        wqA = A.alloc([128, KC, 1024], BF16)
        wqB = A.alloc([128, KC, 385], BF16)
        b_wA, b_wB = P.buf("wqA"), P.buf("wqB")
        hb = [A.alloc([128, KC, 512], BF16) for _ in range(2)]
        b_hb = P.bufs(2, "hblk")
        sf = [A.alloc([128, 512], F32) for _ in range(4)]
        b_sf = P.bufs(4, "sf")
        st = [A.alloc([128, 385], F32) for _ in range(2)]
        b_st = P.bufs(2, "st")
        hv = hT.ap().rearrange("(k p) t -> p k t", p=128)
        step = max(1, KC // 8)

        def loadW(c, dst, bdst, c0, c1):
            wsrc = I["win"][l, c].rearrange("(k p) c -> p k c", p=128)
            for k0 in range(0, KC, step):
                P.dma("pool", dst[:, k0:k0 + step, :], wsrc[:, k0:k0 + step, c0:c1], writes=[bdst])
        steps = [(c, part, tb) for c in range(8) for part in range(2) for tb in range(NBK)]

        def loadH(i):
            tb = steps[i][2]
            P.dma("sp", hb[i % 2], hv[:, :, tb * 512:(tb + 1) * 512], reads=[db["hT"]], writes=[b_hb[i % 2]])
        loadW(0, wqA, b_wA, 0, 1024)
        loadW(0, wqB, b_wB, 1024, 1409)
        loadH(0)
        ci = 0
        for i, (c, part, tb) in enumerate(steps):
            if i + 1 < len(steps):
                loadH(i + 1)
            hk = hb[i % 2]
            bh = b_hb[i % 2]
            if part == 0:
                for cb in range(8):
                    pt, pb = nextps()
                    for kc in range(KC):
                        mm(pt[:, :], wqA[:, kc, cb * 128:(cb + 1) * 128], hk[:, kc, :], kc == 0, kc == KC - 1,
                           [b_wA, bh], [pb])
                    j = ci % 4
                    ci += 1
                    cp("act" if ci % 2 else "dve", sf[j], pt[:, :], [pb], [b_sf[j]])
                    P.dma("sp", PF[c, cb * 128:(cb + 1) * 128, tb * 512:(tb + 1) * 512], sf[j], reads=[b_sf[j]],
                          writes=[db["PF"]])
                if tb == NBK - 1 and c + 1 < 8:
                    loadW(c + 1, wqA, b_wA, 0, 1024)
            else:
                for t4 in range(4):
                    pt, pb = nextps()
                    for kc in range(KC):
                        mm(pt[:, 0:385], hk[:, kc, t4 * 128:(t4 + 1) * 128], wqB[:, kc, :], kc == 0,
                           kc == KC - 1, [b_wB, bh], [pb])
                    j = t4 % 2
                    cp("act" if t4 % 2 else "dve", st[j], pt[:, 0:385], [pb], [b_st[j]])
                    r0 = tb * 512 + t4 * 128
                    P.dma("sp", PT[c, r0:r0 + 128, :], st[j][:, 0:384], reads=[b_st[j]], writes=[db["PT"]])
                    P.dma("sp", PTf[c, :, r0 // 128:r0 // 128 + 1], st[j][:, 384:385], reads=[b_st[j]],
                          writes=[db["PTf"]], slow=True)
                if tb == NBK - 1 and c + 1 < 8:
                    loadW(c + 1, wqB, b_wB, 1024, 1409)
        P.barrier()
        A.release(m0)

    def gelu(eng, out, x, t1, rx, wout_b, b_t1):
        tt(eng, t1, x, x, ALU.mult, rx, [b_t1])
        ts(eng, t1, t1, 0.044715, 1.0, ALU.mult, ALU.add, [b_t1], [b_t1])
        tt(eng, t1, t1, x, ALU.mult, rx + [b_t1], [b_t1])
        act(t1, t1, AF.Sigmoid, [b_t1], [b_t1], scale=1.5957691216)
        tt(eng, out, t1, x, ALU.mult, rx + [b_t1], [wout_b])

    def phase_sgu_stats(l):
        m0 = A.mark()
        junk = A.alloc([128, 128], F32)
        ssq = A.alloc([128, 8, NCL], F32)
        srt = A.alloc([128, NCL], F32)
        b_ssq, b_srt = P.buf("ssq"), P.buf("srt")
        pend = []

        def gen(c):
            gv = A.alloc([128, NCH, 128], F32)
            t1 = A.alloc([128, NCH, 128], F32)
            jk = A.alloc([128, 128], F32)
            b_gv, b_t1, b_junk, b_sq1 = P.buf("gv"), P.buf("t1"), P.buf("junk"), P.buf("sq1")
            for bl in range(BL):
                src = PT[c, bl * S:(bl + 1) * S, 128:256].rearrange("(n m) e -> m n e", m=128)
                P.dma("sp", gv, src, reads=[db["PT"]], writes=[b_gv])
                yield
                gelu("dve", gv, gv, t1, [b_gv], b_gv, b_t1)
                yield
                for n in range(NCH):
                    act(jk, gv[:, n, :], AF.Square, [b_gv], [b_junk, b_sq1],
                        accum=ssq[:, c, bl * NCH + n:bl * NCH + n + 1])
                P.dma("sp", src, gv, reads=[b_gv], writes=[db["PT"]])
                yield
            pend.append(b_sq1)
        for c0 in range(0, 8, 2):
            m1 = A.mark()
            del pend[:]
            wcast_slice(c0 // 2, 20)
            run_gens([gen(c0), gen(c0 + 1)])
            if c0 == 6:
                P.op("dve", lambda e: e.tensor_reduce(out=srt, in_=ssq.rearrange("p c x -> p x c"), axis=AX.X,
                                                      op=ALU.add), list(pend), [b_srt])
                act(srt, srt, AF.Sqrt, [b_srt, b_eps], [b_srt], bias=eps_ap(1e-6), scale=1.0 / 1024)
                recip(srs, srt, [b_srt], [b_srs])
            P.barrier()
            A.release(m1)
        A.release(m0)

    def load_head(l, c, rot, b_rot):
        hc = A.alloc([128, 258], F32)
        lrup = A.alloc([128, 8], F32)
        lw = A.alloc([128, 2, 128], BF16)
        sgbc = A.alloc([128, 2, 128], F32)
        sgw = A.alloc([128, 128], F32)
        sgwb = A.alloc([128, 128], BF16)
        foxp = A.alloc([128, 3], F32)
        b_pp, b_sgwb = P.buf("pp"), P.buf("sgwb")
        P.dma("sp", hc, I["hcst"][c], writes=[b_pp])
        o8 = (l * 8 + c) * 8
        P.dma("sp", lrup, I["lrup"][:, o8:o8 + 8], writes=[b_pp])
        lwf = A.alloc([128, 2, 128], F32)
        b_lwf = P.buf("lwf")
        P.dma("sp", lwf, I["lruw"][l, c].rearrange("t i j -> i t j"), writes=[b_lwf])
        b_lw = P.buf("lw")
        cp("dve", lw, lwf, [b_lwf], [b_lw])
        o2 = (l * 8 + c) * 256
        P.dma("sp", sgbc, I["sgbc"][:, o2:o2 + 256].rearrange("p (t x) -> p t x", t=2), writes=[b_pp])
        P.dma("sp", sgw, I["sgwT"][l, c], writes=[b_pp])
        o3 = (l * 8 + c) * 3
        P.dma("sp", foxp, I["foxp"][:, o3:o3 + 3], writes=[b_pp])
        decT_f, qdbc_f, kd_f, cd_f = hc[:, 0:128], hc[:, 128:256], hc[:, 256:257], hc[:, 257:258]
        tt("dve", sgwb, sgw, triu_f, ALU.mult, [b_pp, b_cst], [b_sgwb])
        m8 = A.alloc([128, 2], F32)
        b_m8 = P.buf("m8")
        act(m8[:, 0:1], lrup[:, 7:8], AF.Exp, [b_pp], [b_m8], scale=-1.0)
        act(m8[:, 0:1], m8[:, 0:1], AF.Ln, [b_m8, b_eps], [b_m8], bias=eps_ap(1.0))
        ts("dve", m8[:, 1:2], m8[:, 0:1], -8.0, None, ALU.mult, None, [b_m8], [b_m8])
        nfb = A.alloc([128, 1], F32)
        b_nfb = P.buf("nfb")
        ts("dve", nfb, foxp[:, 2:3], -1.0, None, ALU.mult, None, [b_pp], [b_nfb])
        return dict(locals())

    def g_sgu(l, c, hp, hi):
        lrup, lw, sgbc, sgwb, foxp, m8, nfb = [hp[k] for k in ["lrup", "lw", "sgbc", "sgwb", "foxp", "m8", "nfb"]]
        b_pp, b_sgwb, b_m8, b_nfb = hp["b_pp"], hp["b_sgwb"], hp["b_m8"], hp["b_nfb"]
        yv = yT.ap()
        PFv = PF[c]
        PTv = PT[c]

        def yrow(mix):
            r0 = mix * 1024 + c * 128
            return slice(r0, r0 + 128)
        gv = A.alloc([128, NCH, 128], F32)
        vb = A.alloc([128, NCH, 128], BF16)
        uT = A.alloc([128, S], F32)
        ut1 = A.alloc([128, S], F32)
        yo = A.alloc([128, S], BF16)
        b_gv, b_vb, b_uT, b_ut1, b_yo = P.buf("gv"), P.buf("vb"), P.buf("uT"), P.buf("ut1"), P.buf("yo")
        for bl in range(BL):
            bs = slice(bl * S, (bl + 1) * S)
            sl = slice(bl * NCH, (bl + 1) * NCH)
            P.dma("sp", gv, PTv[bs, 128:256].rearrange("(n m) e -> m n e", m=128), reads=[db["PT"]], writes=[b_gv])
            tt("dve", gv, gv, srs[:, sl].unsqueeze(2).to_broadcast([128, NCH, 128]), ALU.mult, [b_gv, b_srs], [b_gv])
            tt("dve", vb, gv, sgbc[:, 0, :].unsqueeze(1).to_broadcast([128, NCH, 128]), ALU.mult, [b_gv, b_pp], [b_vb])
            P.dma("sp", uT, PFv[5 * 128:6 * 128, bs], reads=[db["PF"]], writes=[b_uT])
            gelu("dve", uT, uT, ut1, [b_uT], b_uT, b_ut1)
            for q4 in range(NCH // 4):
                pt, pb = nextps()
                for i in range(4):
                    n = q4 * 4 + i
                    mm(pt[:, i * 128:(i + 1) * 128], vb[:, n, :], sgwb, True, True, [b_vb, b_sgwb], [pb])
                tsl = slice(q4 * 512, (q4 + 1) * 512)
                tt("dve", ut1[:, tsl].rearrange("p (a t) -> p a t", a=4), pt[:, :].rearrange("p (a t) -> p a t", a=4),
                   sgbc[:, 1, :].unsqueeze(1).to_broadcast([128, 4, 128]), ALU.add, [pb, b_pp, b_ut1], [b_ut1])
                tt("dve", yo[:, tsl], ut1[:, tsl], uT[:, tsl], ALU.mult, [b_ut1, b_uT], [b_yo])
                yield
            P.dma("sp", yv[yrow(2), bs], yo, reads=[b_yo], writes=[db["yT"]])
            yield

    def g_lru(l, c, hp, hi):
        lrup, lw, sgbc, sgwb, foxp, m8, nfb = [hp[k] for k in ["lrup", "lw", "sgbc", "sgwb", "foxp", "m8", "nfb"]]
        b_pp, b_sgwb, b_m8, b_nfb = hp["b_pp"], hp["b_sgwb"], hp["b_m8"], hp["b_nfb"]
        yv = yT.ap()
        PFv = PF[c]
        PTv = PT[c]

        def yrow(mix):
            r0 = mix * 1024 + c * 128
            return slice(r0, r0 + 128)
        x = A.alloc([128, S], F32)
        xc = A.alloc([128, S], F32)
        xcb = A.alloc([128, S], BF16)
        rg = A.alloc([128, S], F32)
        ig = A.alloc([128, S], F32)
        gt = A.alloc([128, S], F32)
        gt1 = A.alloc([128, S], F32)
        yo = A.alloc([128, S], BF16)
        b_x, b_xc, b_xcb, b_rg, b_ig, b_gt, b_gt1, b_yo = [P.buf(n) for n in
                                                            ["x", "xc", "xcb", "rg", "ig", "gt", "gt1", "yo"]]
        for bl in range(BL):
            bs = slice(bl * S, (bl + 1) * S)
            P.dma("sp", x, PFv[4 * 128:5 * 128, bs], reads=[db["PF"]], writes=[b_x])
            P.dma("sp", gt, PFv[3 * 128:4 * 128, bs], reads=[db["PF"]], writes=[b_gt])
            ts("dve", xc, x, lrup[:, 3:4], lrup[:, 4:5], ALU.mult, ALU.add, [b_x, b_pp], [b_xc])
            for j in range(3):
                sh = 3 - j
                stt(xc[:, sh:S], x[:, 0:S - sh], lrup[:, j:j + 1], xc[:, sh:S], ALU.mult, ALU.add,
                    [b_x, b_pp, b_xc], [b_xc])
            cp("act", xcb, xc, [b_xc], [b_xcb])
            yield
            for q in range(S // 512):
                tsl = slice(q * 512, (q + 1) * 512)
                pt, pb = nextps()
                mm(pt[:, :], lw[:, 0, :], xcb[:, tsl], True, True, [hp["b_lw"], b_xcb], [pb])
                act(rg[:, tsl], pt[:, :], AF.Sigmoid, [pb, b_pp], [b_rg], bias=lrup[:, 5:6])
                pt2, pb2 = nextps()
                mm(pt2[:, :], lw[:, 1, :], xcb[:, tsl], True, True, [hp["b_lw"], b_xcb], [pb2])
                act(ig[:, tsl], pt2[:, :], AF.Sigmoid, [pb2, b_pp], [b_ig], bias=lrup[:, 6:7])
                yield
            act(rg, rg, AF.Exp, [b_rg, b_m8], [b_rg], scale=m8[:, 1:2])
            tt("dve", ig, ig, xc, ALU.mult, [b_ig, b_xc], [b_ig])
            yield
            tt("dve", x, rg, rg, ALU.mult, [b_rg], [b_x])
            ts("dve", x, x, -1.0, 1.0, ALU.mult, ALU.add, [b_x], [b_x])
            ts("dve", x, x, 0.0, None, ALU.max, None, [b_x], [b_x])
            act(x, x, AF.Sqrt, [b_x], [b_x])
            yield
            tt("dve", ig, ig, x, ALU.mult, [b_ig, b_x], [b_ig])
            P.op("dve", lambda e: e.tensor_tensor_scan(out=xc, data0=rg, data1=ig, initial=0.0, op0=ALU.mult,
                                                       op1=ALU.add), [b_rg, b_ig], [b_xc])
            gelu("dve", gt, gt, gt1, [b_gt], b_gt, b_gt1)
            tt("dve", yo, xc, gt, ALU.mult, [b_xc, b_gt], [b_yo])
            P.dma("sp", yv[yrow(1), bs], yo, reads=[b_yo], writes=[db["yT"]])
            yield

    def g_ret(l, c, hp, hi):
        rot, b_rot = hp["rot"], hp["b_rot"]
        decT_f, qdbc_f, kd_f, cd_f = hp["decT_f"], hp["qdbc_f"], hp["kd_f"], hp["cd_f"]
        b_pp = hp["b_pp"]
        yv = yT.ap()
        PFv = PF[c]
        PTv = PT[c]

        def yrow(mix):
            r0 = mix * 1024 + c * 128
            return slice(r0, r0 + 128)
        q = A.alloc([128, S], F32)
        kk = A.alloc([128, S], F32)
        tmp = A.alloc([128, S], F32)
        qr = A.alloc([128, S], BF16)
        kr = A.alloc([128, S], BF16)
        qs = A.alloc([128, S], BF16)
        vtm = A.alloc([128, NCH, 128], BF16)
        vst = A.alloc([128, NCH, 128], F32)
        b_vst = P.buf("vst")
        g = A.alloc([128, S], F32)
        yr = A.alloc([128, S], F32)
        ysq = A.alloc([128, 512], F32)
        yo = A.alloc([128, S], BF16)
        prev = A.alloc([128, 128], F32)
        prevb = A.alloc([128, 128], BF16)
        sT = [A.alloc([128, 128], BF16) for _ in range(2)]
        kd = [A.alloc([128, 128], BF16) for _ in range(2)]
        rsr = A.alloc([128, 512], F32)
        (b_q, b_k, b_tmp, b_qr, b_kr, b_qs, b_v, b_g, b_yr, b_ysq, b_yo, b_prev, b_prevb, b_rsr) = [
            P.buf(n) for n in ["q", "k", "tmp", "qr", "kr", "qs", "v", "g", "yr", "ysq", "yo", "prev", "prevb",
                               "rsr"]]
        b_sT = P.bufs(2, "sT")
        b_kd = P.bufs(2, "kd")
        cosT, sinT = rot[:, 0, :], rot[:, 1, :]

        def rotary(src, dst_b, bsrc, bdst, scale):
            for qq in range(S // 512):
                tsl = slice(qq * 512, (qq + 1) * 512)
                pt, pb = nextps()
                mm(pt[:, :], Pm_f, src[:, tsl], True, True, [b_cst, bsrc], [pb])
                tt("dve", tmp[:, tsl], pt[:, :], sinT[:, tsl], ALU.mult, [pb, b_rot], [b_tmp])
            tt("dve", src, src, cosT, ALU.mult, [bsrc, b_rot], [bsrc])
            if scale == 1.0:
                tt("dve", dst_b, src, tmp, ALU.add, [bsrc, b_tmp], [bdst])
            else:
                tt("dve", src, src, tmp, ALU.add, [bsrc, b_tmp], [bsrc])
                ts("dve", dst_b, src, scale, None, ALU.mult, None, [bsrc], [bdst])

        for bl in range(BL):
            bs = slice(bl * S, (bl + 1) * S)
            P.dma("sp", q, PFv[0:128, bs], reads=[db["PF"]], writes=[b_q])
            P.dma("sp", kk, PFv[128:256, bs], reads=[db["PF"]], writes=[b_k])
            P.dma("sp", g, PFv[256:384, bs], reads=[db["PF"]], writes=[b_g])
            P.dma("sp", vst, PTv[bs, 0:128].rearrange("(n m) e -> m n e", m=128), reads=[db["PT"]], writes=[b_vst])
            cp("act", vtm, vst, [b_vst], [b_v])
            rotary(q, qr, b_q, b_qr, 1.0)
            yield
            rotary(kk, kr, b_k, b_kr, float(HD ** -0.5))
            yield
            tt("dve", qs.rearrange("p (n c) -> p n c", c=128), qr.rearrange("p (n c) -> p n c", c=128),
               qdbc_f.unsqueeze(1).to_broadcast([128, NCH, 128]), ALU.mult, [b_qr, b_pp], [b_qs])
            P.op("dve", lambda e: e.memset(prev, 0.0), [], [b_prev])
            P.op("dve", lambda e: e.memset(prevb, 0.0), [], [b_prevb])
            act(g, g, AF.Silu, [b_g], [b_g])
            for q4 in range(NCH // 4):
                po, pob = accps(hi)
                for i in range(4):
                    n = q4 * 4 + i
                    csl = slice(n * 128, (n + 1) * 128)
                    j = n % 2
                    p1, p1b = nextps()
                    mm(p1[:, 0:128], kr[:, csl], qr[:, csl], True, True, [b_kr, b_qr], [p1b])
                    tt("dve", sT[j], p1[:, 0:128], decT_f, ALU.mult, [p1b, b_pp], [b_sT[j]])
                    p2, p2b = nextps()
                    p2v = p2[:, :].bitcast(BF16)
                    tr(p2v[:, 0:128], kr[:, csl], ident_b, [b_kr, b_cstb], [p2b])
                    ts("dve", kd[j], p2v[:, 0:128], kd_f, None, ALU.mult, None, [p2b, b_pp], [b_kd[j]])
                    mm(po[:, i * 128:(i + 1) * 128], vtm[:, n, :], sT[j], True, False, [b_v, b_sT[j]], [pob])
                    mm(po[:, i * 128:(i + 1) * 128], prevb, qs[:, csl], False, True, [b_prevb, b_qs], [pob])
                    p3, p3b = nextps()
                    mm(p3[:, 0:128], kd[j], vtm[:, n, :], True, True, [b_kd[j], b_v], [p3b])
                    stt(prev, prev, cd_f, p3[:, 0:128], ALU.mult, ALU.add, [b_prev, b_pp, p3b], [b_prev])
                    cp("act", prevb, prev, [b_prev], [b_prevb])
                    yield
                tsl = slice(q4 * 512, (q4 + 1) * 512)
                cp("act", yr[:, tsl], po[:, :], [pob], [b_yr])
                tt("dve", ysq, yr[:, tsl], yr[:, tsl], ALU.mult, [b_yr], [b_ysq])
                pn, pnb = nextps()
                mm(pn[:, :], ones_f, ysq, True, True, [b_cst, b_ysq], [pnb])
                act(rsr, pn[:, :], AF.Sqrt, [pnb, b_eps], [b_rsr], bias=eps_ap(1e-6), scale=1.0 / HD)
                recip(rsr, rsr, [b_rsr], [b_rsr])
                tt("dve", yr[:, tsl], yr[:, tsl], rsr, ALU.mult, [b_yr, b_rsr], [b_yr])
                tt("dve", yo[:, tsl], yr[:, tsl], g[:, tsl], ALU.mult, [b_yr, b_g], [b_yo])
                yield
            P.dma("sp", yv[yrow(0), bs], yo, reads=[b_yo], writes=[db["yT"]])
            yield

    def g_fox(l, c, hp, hi):
        foxp, nfb, b_pp, b_nfb = hp["foxp"], hp["nfb"], hp["b_pp"], hp["b_nfb"]
        yv = yT.ap()
        PFv = PF[c]
        PTv = PT[c]

        def yrow(mix):
            r0 = mix * 1024 + c * 128
            return slice(r0, r0 + 128)
        q = A.alloc([128, S], F32)
        kk = A.alloc([128, S], F32)
        tmp = A.alloc([128, S], F32)
        qn = A.alloc([128, S], BF16)
        kn = A.alloc([128, S], BF16)
        vtm = A.alloc([128, NCH, 128], BF16)
        vst = A.alloc([128, NCH, 128], F32)
        b_vst = P.buf("vst")
        ff = A.alloc([128, NCH], F32)
        lf = A.alloc([128, NCH], F32)
        cum = A.alloc([128, NCH], F32)
        car = A.alloc([128, NCH], F32)
        tot = A.alloc([128, NCH], F32)
        onesn = A.alloc([128, NCH], F32)
        bias = A.alloc([128, NCH, NCH], F32)
        pT = [A.alloc([128, 512], BF16) for _ in range(3)]
        rden = A.alloc([128, 512], F32)
        yo = A.alloc([128, S], BF16)
        rsq = A.alloc([128, 512], F32)
        (b_q, b_k, b_tmp, b_qn, b_kn, b_v, b_ff, b_lf, b_cum, b_car, b_tot, b_on, b_bias, b_rden, b_yo,
         b_rsq) = [P.buf(n) for n in ["q", "k", "tmp", "qn", "kn", "v", "ff", "lf", "cum", "car", "tot", "on",
                                       "bias", "rden", "yo", "rsq"]]
        b_pT = P.bufs(3, "pT")
        P.op("dve", lambda e: e.memset(onesn, 1.0), [], [b_on])

        def qknorm(src, dst_b, bsrc, bdst, gcol, scale):
            for qq in range(S // 512):
                tsl = slice(qq * 512, (qq + 1) * 512)
                tt("dve", tmp[:, tsl], src[:, tsl], src[:, tsl], ALU.mult, [bsrc], [b_tmp])
                pt, pb = nextps()
                mm(pt[:, :], ones_f, tmp[:, tsl], True, True, [b_cst, b_tmp], [pb])
                act(rsq, pt[:, :], AF.Sqrt, [pb, b_eps], [b_rsq], bias=eps_ap(1e-6), scale=1.0 / HD)
                recip(rsq, rsq, [b_rsq], [b_rsq])
                stt(tmp[:, tsl], src[:, tsl], foxp[:, gcol:gcol + 1], rsq, ALU.mult, ALU.mult, [bsrc, b_pp, b_rsq],
                    [b_tmp])
            ts("dve", dst_b, tmp, scale, None, ALU.mult, None, [b_tmp], [bdst])

        pi = 0
        for bl in range(BL):
            bs = slice(bl * S, (bl + 1) * S)
            P.dma("sp", q, PFv[6 * 128:7 * 128, bs], reads=[db["PF"]], writes=[b_q])
            P.dma("sp", kk, PFv[7 * 128:8 * 128, bs], reads=[db["PF"]], writes=[b_k])
            P.dma("sp", vst, PTv[bs, 256:384].rearrange("(n m) e -> m n e", m=128), reads=[db["PT"]], writes=[b_vst])
            cp("act", vtm, vst, [b_vst], [b_v])
            P.dma("sp", ff, PTf[c, :, bl * NCH:(bl + 1) * NCH], reads=[db["PTf"]], writes=[b_ff])
            qknorm(q, qn, b_q, b_qn, 0, float(HD ** -0.5))
            yield
            qknorm(kk, kn, b_k, b_kn, 1, 1.0)
            yield
            act(lf, ff, AF.Exp, [b_ff, b_nfb], [b_lf], bias=nfb, scale=-1.0)
            act(lf, lf, AF.Ln, [b_lf, b_eps], [b_lf], bias=eps_ap(1.0))
            ts("dve", lf, lf, -1.0, None, ALU.mult, None, [b_lf], [b_lf])
            pc, pcb = nextps()
            mm(pc[:, 0:NCH], triu_f, lf, True, True, [b_cst, b_lf], [pcb])
            mm(pc[:, 64:64 + NCH], ones_f, lf, True, True, [b_cst, b_lf], [pcb])
            cp("dve", tot, pc[:, 64:64 + NCH], [pcb], [b_tot])
            P.op("dve", lambda e: e.tensor_tensor_scan(out=car, data0=onesn, data1=tot, initial=0.0, op0=ALU.mult,
                                                       op1=ALU.add), [b_on, b_tot], [b_car])
            tt("dve", car, car, tot, ALU.subtract, [b_car, b_tot], [b_car])
            tt("dve", cum, pc[:, 0:NCH], car, ALU.add, [pcb, b_car], [b_cum])
            for qb in range(NCH):
                ts("dve", bias[:, qb, :], cum, -1.0, car[:, qb:qb + 1], ALU.mult, ALU.add, [b_cum, b_car], [b_bias])
            for sb in range(S // 512):
                po, pob = accps(2 * hi)
                pd, pdb = accps(2 * hi + 1)
                nkt = 4 * sb + 4
                for kt in range(nkt):
                    i0 = max(0, kt - 4 * sb)
                    q0 = sb * 512 + i0 * 128
                    w = 512 - i0 * 128
                    pS, pSb = nextps()
                    mm(pS[:, 0:w], kn[:, kt * 128:(kt + 1) * 128], qn[:, q0:q0 + w], True, True, [b_kn, b_qn], [pSb])
                    j = pi % 3
                    pi += 1
                    for i in range(i0, 4):
                        qb = 4 * sb + i
                        act(pT[j][:, (i - i0) * 128:(i - i0 + 1) * 128], pS[:, (i - i0) * 128:(i - i0 + 1) * 128],
                            AF.Exp, [pSb, b_bias], [b_pT[j]], bias=bias[:, qb, kt:kt + 1])
                    if kt >= 4 * sb:
                        tt("dve", pT[j][:, 0:128], pT[j][:, 0:128], triu_b, ALU.mult, [b_pT[j], b_cstb], [b_pT[j]])
                    mm(po[:, i0 * 128:512], vtm[:, kt, :], pT[j][:, 0:w], kt == 0, kt == nkt - 1, [b_v, b_pT[j]],
                       [pob])
                    mm(pd[:, i0 * 128:512], ones_b, pT[j][:, 0:w], kt == 0, kt == nkt - 1, [b_cstb, b_pT[j]], [pdb])
                    yield
                recip(rden, pd[:, :], [pdb], [b_rden])
                tt("dve", yo[:, sb * 512:(sb + 1) * 512], po[:, :], rden, ALU.mult, [pob, b_rden], [b_yo])
            P.dma("sp", yv[yrow(3), bs], yo, reads=[b_yo], writes=[db["yT"]])
            yield

    def phase_mixers_all(l):
        m0 = A.mark()
        rot = A.alloc([128, 2, S], F32)
        b_rot = P.buf("rot")
        P.dma("sp", rot, I["rot"].rearrange("p (t s) -> p t s", t=2), writes=[b_rot])
        for c0 in range(0, 8, 2):
            mA = A.mark()
            hps = [load_head(l, c0 + i, rot, b_rot) for i in range(2)]
            for gi, gen in enumerate((g_sgu, g_lru, g_ret, g_fox)):
                m1 = A.mark()
                psmode[0] = 4 if gen is g_fox else 6
                psi[0] = 0
                wcast_slice(4 + (c0 // 2) * 4 + gi, 20)
                run_gens([gen(l, c0 + i, hps[i], i) for i in range(2)])
                P.barrier()
                psmode[0] = 6
                A.release(m1)
            A.release(mA)
        A.release(m0)

    def phase_outproj(l, src_ap, src_buf):
        m0 = A.mark()
        FG = min(8, KC)
        wo = A.alloc([128, 32, FG * 128], BF16)
        b_wo = P.buf("wo")
        yb = [A.alloc([128, 32, 512], BF16) for _ in range(2)]
        b_yb = P.bufs(2, "yb")
        xk = [A.alloc([128, 512], F32) for _ in range(3)]
        b_xk = P.bufs(3, "xk")
        yvv = yT.ap().rearrange("(k p) t -> p k t", p=128)
        wv = I["wout"][l].rearrange("(k p) d -> p k d", p=128)
        sv = src_ap.rearrange("(k p) t -> p k t", p=128)
        dv = xres.ap().rearrange("(k p) t -> p k t", p=128)
        ci = 0
        li = 0
        for fg in range(KC // FG):
            for k8 in range(0, 32, 8):
                P.dma("pool", wo[:, k8:k8 + 8, :], wv[:, k8:k8 + 8, fg * FG * 128:(fg + 1) * FG * 128], writes=[b_wo])
            for tb in range(NBK):
                bl = tb // BPB
                tsl = slice(tb * 512, (tb + 1) * 512)
                u = li % 2
                li += 1
                P.dma("sp", yb[u], yvv[:, :, tsl], reads=[db["yT"]], writes=[b_yb[u]])
                for f8 in range(FG):
                    fo = fg * FG + f8
                    i = ci % 3
                    ci += 1
                    P.dma("sp", xk[i], sv[:, fo, tsl], reads=[src_buf], writes=[b_xk[i]])
                    pt, pb = nextps()
                    for kq in range(32):
                        mm(pt[:, :], wo[:, kq, f8 * 128:(f8 + 1) * 128], yb[u][:, kq, :], kq == 0, kq == 31,
                           [b_wo, b_yb[u]], [pb])
                    stt(xk[i], pt[:, :], MV(bl, l, 2)[:, fo:fo + 1], xk[i], ALU.mult, ALU.add,
                        [pb, b_xk[i], b_modv], [b_xk[i]])
                    P.dma("sp", dv[:, fo, tsl], xk[i], reads=[b_xk[i]], writes=[db["xres"]])
        P.barrier()
        A.release(m0)

    def make_moe_extra(l):
        st = {}

        def extra(kind, args):
            if kind == "alloc":
                st["rwb"] = A.alloc([128, KC, E], BF16)
                st["rb"] = A.alloc([128, E], F32)
                st["b_rw"] = P.buf("rw")
                P.dma("pool", st["rwb"], I["rw"][l].rearrange("(k p) e -> p k e", p=128), writes=[st["b_rw"]])
                P.dma("sp", st["rb"], I["rbbc"][:, l * E:(l + 1) * E], writes=[st["b_rw"]])
                st["sc"] = A.alloc([128, E], F32)
                st["sel"] = A.alloc([128, E], F32)
                st["top"] = A.alloc([128, 8], F32)
                st["den"] = A.alloc([128, 1], F32)
                st["gts"] = A.alloc([128, 4, E], F32)
                st["gT"] = A.alloc([E, 512], F32)
                for n in ["sc", "sel", "top", "den", "gts", "gT"]:
                    st["b_" + n] = P.buf(n)
                st["swg"] = A.alloc([128, KC, 256], BF16)
                st["swu"] = A.alloc([128, KC, 256], BF16)
                st["swd"] = A.alloc([128, 2, D], BF16)
                st["b_sw"] = P.buf("sw")
                P.dma("pool", st["swg"], I["swg"][l].rearrange("(k p) f -> p k f", p=128), writes=[st["b_sw"]])
                P.dma("pool", st["swu"], I["swu"][l].rearrange("(k p) f -> p k f", p=128), writes=[st["b_sw"]])
                P.dma("pool", st["swd"], I["swd"][l].rearrange("(f p) d -> p f d", p=128), writes=[st["b_sw"]])
                st["sg"] = A.alloc([128, 512], F32)
                st["hid"] = A.alloc([128, 2, 512], BF16)
                st["b_sg"], st["b_hid"] = P.buf("sg"), P.buf("hid")
                st["xs"] = [A.alloc([128, 512], F32) for _ in range(2)]
                st["b_xs"] = P.bufs(2, "xs")
                return st
            if kind == "post":
                tb, bl, hb, b_h = args
                for t4 in range(4):
                    pt, pb = nextps()
                    for kc in range(KC):
                        mm(pt[:, 0:E], hb[:, kc, t4 * 128:(t4 + 1) * 128], st["rwb"][:, kc, :], kc == 0, kc == KC - 1,
                           [b_h, st["b_rw"]], [pb])
                    act(st["sc"], pt[:, 0:E], AF.Sigmoid, [pb], [st["b_sc"]])
                    tt("dve", st["sel"], st["sc"], st["rb"], ALU.add, [st["b_sc"], st["b_rw"]], [st["b_sel"]])
                    P.op("dve", lambda e: e.max(out=st["top"], in_=st["sel"]), [st["b_sel"]], [st["b_top"]])
                    ts("dve", st["sel"], st["sel"], st["top"][:, 7:8], None, ALU.is_ge, None,
                       [st["b_sel"], st["b_top"]], [st["b_sel"]])
                    tt("dve", st["sel"], st["sel"], st["sc"], ALU.mult, [st["b_sel"], st["b_sc"]], [st["b_sel"]])
                    P.op("dve", lambda e: e.tensor_reduce(out=st["den"], in_=st["sel"], axis=AX.X, op=ALU.add),
                         [st["b_sel"]], [st["b_den"]])
                    recip(st["den"], st["den"], [st["b_den"]], [st["b_den"]])
                    ts("dve", st["gts"][:, t4, :], st["sel"], st["den"][:, 0:1], 2.5, ALU.mult, ALU.mult,
                       [st["b_sel"], st["b_den"]], [st["b_gts"]])
                pt, pb = nextps()
                for t4 in range(4):
                    tr(pt[0:E, t4 * 128:(t4 + 1) * 128], st["gts"][:, t4, :], ident_f, [st["b_gts"], b_cst], [pb])
                cp("dve", st["gT"], pt[0:E, :], [pb], [st["b_gT"]])
                P.dma("sp", gTd.ap()[:, tb * 512:(tb + 1) * 512], st["gT"], reads=[st["b_gT"]], writes=[db["gTd"]])
                for f in range(2):
                    pg, pgb = nextps()
                    pu, pub = nextps()
                    for kc in range(KC):
                        mm(pg[:, :], st["swg"][:, kc, f * 128:(f + 1) * 128], hb[:, kc, :], kc == 0, kc == KC - 1,
                           [st["b_sw"], b_h], [pgb])
                    for kc in range(KC):
                        mm(pu[:, :], st["swu"][:, kc, f * 128:(f + 1) * 128], hb[:, kc, :], kc == 0, kc == KC - 1,
                           [st["b_sw"], b_h], [pub])
                    act(st["sg"], pg[:, :], AF.Silu, [pgb], [st["b_sg"]])
                    tt("dve", st["hid"][:, f, :], st["sg"], pu[:, :], ALU.mult, [st["b_sg"], pub], [st["b_hid"]])
                xs = st["xs"]
                for fo in range(KC):
                    po, pob = nextps()
                    for f in range(2):
                        mm(po[:, :], st["swd"][:, f, fo * 128:(fo + 1) * 128], st["hid"][:, f, :], f == 0, f == 1,
                           [st["b_sw"], st["b_hid"]], [pob])
                    i = fo % 2
                    ts("dve", xs[i], po[:, :], MV(bl, l, 5)[:, fo:fo + 1], None, ALU.mult, None,
                       [pob, b_modv], [st["b_xs"][i]])
                    P.dma("sp", shr.ap()[fo * 128:(fo + 1) * 128, tb * 512:(tb + 1) * 512], xs[i],
                          reads=[st["b_xs"][i]], writes=[db["shr"]])
                return None
        return extra

    def wcast_jobs(l):
        jobs = []
        for e in range(E):
            jobs.append(lambda e=e: P.dma("pool", wgb.ap()[e], I["ewg"][l, e], writes=[db["wgb"]], bg=True))
            jobs.append(lambda e=e: P.dma("pool", wub.ap()[e], I["ewu"][l, e], writes=[db["wub"]], bg=True))
        for g in range(NG):
            src = I["ewd"][l, g * 8:(g + 1) * 8].rearrange("j (f p) (dg c) -> dg p j f c", p=128, c=256)
            dst = wdb.ap()[g].rearrange("dg p (j f c) -> dg p j f c", j=8, f=2)
            for dg in range(DG):
                jobs.append(lambda src=src, dst=dst, dg=dg: P.dma("pool", dst[dg], src[dg], writes=[db["wdb"]],
                                                                  bg=True))
        return jobs

    wjobs = []

    def wcast_slice(k, n):
        tot = len(wjobs)
        for j in range(k * tot // n, (k + 1) * tot // n):
            wjobs[j]()

    def phase_moe(l, dst_ap, dst_buf):
        m0 = A.mark()
        acc = A.alloc([128, KC, 512], F32)
        hbk = A.alloc([128, KC, 512], BF16)
        hid = A.alloc([128, 16, 512], BF16)
        gsel = A.alloc([E, 8 * 128], F32)
        gblk = A.alloc([E, 512], F32)
        sg = [A.alloc([128, 512], F32) for _ in range(2)]
        wd = [A.alloc([128, 16, 256], BF16) for _ in range(2)]
        mW = A.mark()
        wg = [A.alloc([128, KC, 128], BF16) for _ in range(2)]
        wu = [A.alloc([128, KC, 128], BF16) for _ in range(2)]
        A.release(mW)
        KG = 4 if KC >= 4 else KC
        x1k = [A.alloc([128, KG, 512], F32) for _ in range(2)]
        shk = [A.alloc([128, KG, 512], F32) for _ in range(2)]
        hv = hT.ap().rearrange("(k p) t -> p k t", p=128)
        xv = xres.ap().rearrange("(k p) t -> p k t", p=128)
        sv = shr.ap().rearrange("(k p) t -> p k t", p=128)
        dv = dst_ap.rearrange("(k p) t -> p k t", p=128)
        wgv = wgb.ap().rearrange("e f p (k c) -> e f p k c", c=128)
        wuv = wub.ap().rearrange("e f p (k c) -> e f p k c", c=128)
        wdv = wdb.ap().rearrange("g dg p (q c) -> g dg p q c", c=256)
        for tb in range(NBK):
            bl = tb // BPB
            tsl = slice(tb * 512, (tb + 1) * 512)
            b_acc, b_hbk, b_hid, b_gsel, b_gblk = [P.buf(n) for n in ["acc", "hbk", "hid", "gsel", "gblk"]]
            b_sg = P.bufs(2, "sg")
            b_wd = P.bufs(2, "wd")
            b_wg = P.bufs(2, "wg")
            b_wu = P.bufs(2, "wu")
            P.dma("sp", hbk, hv[:, :, tsl], reads=[db["hT"]], writes=[b_hbk])
            P.dma("sp", gblk, gTd.ap()[:, tsl], reads=[db["gTd"]], writes=[b_gblk])
            wi = 0
            di = 0
            for g in range(NG):
                P.dma("sp", gsel, I["selB"][g], writes=[b_gsel])
                for j in range(8):
                    e = g * 8 + j
                    for f in range(2):
                        w = wi % 2
                        wi += 1
                        P.dma("sp", wg[w], wgv[e, f], reads=[db["wgb"]], writes=[b_wg[w]])
                        P.dma("sp", wu[w], wuv[e, f], reads=[db["wub"]], writes=[b_wu[w]])
                        pg, pgb = nextps()
                        pu, pub = nextps()
                        for kc in range(KC):
                            mm(pg[:, :], wg[w][:, kc, :], hbk[:, kc, :], kc == 0, kc == KC - 1, [b_wg[w], b_hbk], [pgb])
                        for kc in range(KC):
                            mm(pu[:, :], wu[w][:, kc, :], hbk[:, kc, :], kc == 0, kc == KC - 1, [b_wu[w], b_hbk], [pub])
                        pb_, pbb = nextps()
                        mm(pb_[:, :], gsel[:, j * 128:(j + 1) * 128], gblk, True, True, [b_gsel, b_gblk], [pbb])
                        s = wi % 2
                        act(sg[s], pg[:, :], AF.Silu, [pgb], [b_sg[s]])
                        tt("dve", sg[s], sg[s], pu[:, :], ALU.mult, [b_sg[s], pub], [b_sg[s]])
                        tt("dve", hid[:, 2 * j + f, :], sg[s], pb_[:, :], ALU.mult, [b_sg[s], pbb], [b_hid])
                for dg in range(DG):
                    w = di % 2
                    di += 1
                    P.dma("sp", wd[w], wdv[g, dg], reads=[db["wdb"]], writes=[b_wd[w]])
                    for f2 in range(2):
                        fo = dg * 2 + f2
                        po, pob = nextps()
                        for kq in range(16):
                            mm(po[:, :], wd[w][:, kq, f2 * 128:(f2 + 1) * 128], hid[:, kq, :], kq == 0, kq == 15,
                               [b_wd[w], b_hid], [pob])
                        if g == 0:
                            cp("act", acc[:, fo, :], po[:, :], [pob], [b_acc])
                        else:
                            tt("dve", acc[:, fo, :], acc[:, fo, :], po[:, :], ALU.add, [b_acc, pob], [b_acc])
            P.barrier()
            b_x1k, b_shk = P.bufs(2, "x1k"), P.bufs(2, "shk")
            it = 0
            for k0 in range(0, KC, KG):
                i = it % 2
                it += 1
                ksl = slice(k0, k0 + KG)
                P.dma("sp", x1k[i], xv[:, ksl, tsl], reads=[db["xres"]], writes=[b_x1k[i]])
                P.dma("sp", shk[i], sv[:, ksl, tsl], reads=[db["shr"]], writes=[b_shk[i]])
                tt("dve", x1k[i], x1k[i], shk[i], ALU.add, [b_x1k[i], b_shk[i]], [b_x1k[i]])
                for kk_ in range(KG):
                    kc = k0 + kk_
                    stt(x1k[i][:, kk_, :], acc[:, kc, :], MV(bl, l, 5)[:, kc:kc + 1], x1k[i][:, kk_, :],
                        ALU.mult, ALU.add, [b_acc, b_x1k[i], b_modv], [b_x1k[i]])
                P.dma("sp", dv[:, ksl, tsl], x1k[i], reads=[b_x1k[i]], writes=[dst_buf])
            P.barrier()
        A.release(m0)

    phase_mod()
    cur_ap, cur_buf = I["xT"], NB
    for l in range(L):
        phase_norm(l, 0, cur_ap, cur_buf)
        phase_inproj_all(l)
        wjobs[:] = wcast_jobs(l)
        phase_sgu_stats(l)
        phase_mixers_all(l)
        phase_outproj(l, cur_ap, cur_buf)
        phase_norm(l, 1, xres.ap(), db["xres"], extra=make_moe_extra(l))
        last = (l == L - 1)
        phase_moe(l, outT if last else xres.ap(), db["outT"] if last else db["xres"])
        cur_ap, cur_buf = xres.ap(), db["xres"]
    P.barrier(final=True)
    P.emit()
    print("prog stats", P.stats, flush=True)
    es.close()
    return nc


def _fm(v, kc):
    return np.ascontiguousarray(np.asarray(v, np.float32).reshape(kc, 128).T)


def host_consts(cfg):
    S = cfg.S
    ident = np.eye(128, dtype=np.float32)
    idx = np.arange(128)
    triu = (idx[:, None] <= idx[None, :]).astype(np.float32)
    ones = np.ones((128, 128), np.float32)
    Pm = np.zeros((128, 128), np.float32)
    for d in range(64):
        Pm[d + 64, d] = -1.0
        Pm[d, d + 64] = 1.0
    cst = np.concatenate([ident, triu, ones, Pm], axis=1).astype(np.float32)
    hc = []
    for c in range(8):
        lg = np.log1p(-2.0 ** (-5.0 - c))
        rel = (idx[None, :] - idx[:, None]).astype(np.float64)
        decT = np.where(rel >= 0, np.exp(rel * lg), 0.0).astype(np.float32)
        qd = np.exp((idx + 1) * lg).astype(np.float32)
        qdbc = np.tile(qd[None, :], (128, 1))
        kd = np.exp((127 - idx) * lg).astype(np.float32)[:, None]
        cd = np.full((128, 1), np.exp(128 * lg), np.float32)
        hc.append(np.concatenate([decT, qdbc, kd, cd], axis=1))
    hcst = np.stack(hc).astype(np.float32)
    half = 64
    inv = (10000.0 ** (-np.arange(half, dtype=np.float32) / half)).astype(np.float32)
    pos = np.arange(S, dtype=np.float32)
    ang = (pos[:, None] * inv[None, :]).astype(np.float32)
    cosT = np.cos(ang).T.astype(np.float32)
    sinT = np.sin(ang).T.astype(np.float32)
    rot = np.concatenate([np.concatenate([cosT, cosT], 0), np.concatenate([sinT, sinT], 0)], axis=1)
    return cst, hcst, np.ascontiguousarray(rot.astype(np.float32))


def make_in_maps(cfg, inp):
    D, S, B, E, L, KC, BL, NTL, NG = cfg.D, cfg.S, cfg.B, cfg.E, cfg.L, cfg.KC, cfg.BL, cfg.NTL, cfg.NG
    f32 = np.float32
    A_ = lambda k: np.asarray(inp[k], f32)
    x = A_("x")
    cvec = A_("c")
    sh = {}
    sh["cT"] = np.ascontiguousarray(cvec.T.reshape(KC, 128, B).transpose(1, 0, 2).reshape(128, KC * B))
    sh["wada"] = A_("w_ada")
    sh["bada"] = _fm(A_("b_ada"), 6 * KC)
    sh["tab"] = np.concatenate([_fm(A_("ada_table")[l].reshape(-1), 6 * KC) for l in range(L)], 1)
    sh["ng"] = np.concatenate([np.concatenate([_fm(A_("norm1_g")[l], KC), _fm(A_("norm2_g")[l], KC)], 1)
                               for l in range(L)], 1)
    w_in = A_("w_in")
    order = [0, 1, 3, 4, 5, 6, 8, 9, 2, 7, 10]
    win = np.empty((L, 8, D, 1409), f32)
    for c in range(8):
        for i, blk in enumerate(order):
            win[:, c, :, i * 128:(i + 1) * 128] = w_in[:, :, blk * 1024 + c * 128: blk * 1024 + (c + 1) * 128]
        win[:, c, :, 1408] = w_in[:, :, 11 * 1024 + c]
    sh["win"] = win
    sh["wout"] = A_("w_out")
    lrup = np.empty((128, L, 8, 8), f32)
    sgbc = np.empty((128, L, 8, 2, 128), f32)
    foxp = np.empty((128, L, 8, 3), f32)
    for l in range(L):
        for c in range(8):
            sl = slice(c * 128, (c + 1) * 128)
            for j in range(4):
                lrup[:, l, c, j] = A_("lru_conv_w")[l, j, sl]
            lrup[:, l, c, 4] = A_("lru_conv_b")[l, sl]
            lrup[:, l, c, 5] = A_("lru_ba")[l, sl]
            lrup[:, l, c, 6] = A_("lru_bx")[l, sl]
            lrup[:, l, c, 7] = A_("lru_lambda")[l, sl]
            sgbc[:, l, c, 0, :] = A_("sgu_norm_g")[l, sl][None, :]
            sgbc[:, l, c, 1, :] = A_("sgu_b")[l, c][None, :]
            foxp[:, l, c, 0] = A_("fox_qn")[l]
            foxp[:, l, c, 1] = A_("fox_kn")[l]
            foxp[:, l, c, 2] = A_("fox_fb")[l, c]
    sh["lrup"] = lrup.reshape(128, -1)
    sh["sgbc"] = sgbc.reshape(128, -1)
    sh["foxp"] = foxp.reshape(128, -1)
    sh["lruw"] = np.ascontiguousarray(np.stack([A_("lru_wa"), A_("lru_wx")], axis=2))
    sh["sgwT"] = np.ascontiguousarray(A_("sgu_w").transpose(0, 1, 3, 2))
    sh["rw"] = A_("router_w")
    sh["rbbc"] = np.ascontiguousarray(np.concatenate([np.tile(A_("router_bias")[l][None, :], (128, 1))
                                                      for l in range(L)], 1))

    def relay(w):
        w = w.reshape(L, E, KC, 128, 2, 128).transpose(0, 1, 4, 3, 2, 5)
        return np.ascontiguousarray(w.reshape(L, E, 2, 128, KC * 128))
    sh["ewg"] = relay(A_("exp_w_gate"))
    sh["ewu"] = relay(A_("exp_w_up"))
    sh["ewd"] = A_("exp_w_down")
    sh["swg"] = A_("sh_w_gate")
    sh["swu"] = A_("sh_w_up")
    sh["swd"] = A_("sh_w_down")
    selB = np.zeros((NG, E, 8 * 128), f32)
    for g in range(NG):
        for j in range(8):
            selB[g, g * 8 + j, j * 128:(j + 1) * 128] = 1.0
    sh["selB"] = selB
    sh["cst"], sh["hcst"], sh["rot"] = host_consts(cfg)
    sh = {k: np.ascontiguousarray(v, dtype=f32) for k, v in sh.items()}
    maps = []
    for c in range(cfg.NCORE):
        m = dict(sh)
        xb = x[c * BL:(c + 1) * BL].reshape(NTL, D)
        m["xT"] = np.ascontiguousarray(xb.T)
        sel = np.zeros((128, BL, B), f32)
        for bl in range(BL):
            sel[:, bl, c * BL + bl] = 1.0
        m["selb"] = sel.reshape(128, BL * B)
        maps.append(m)
    return maps


_NC_CACHE = {}


def run(cfg, inputs):
    key = (cfg.D, cfg.S, cfg.B, cfg.E, cfg.L, cfg.NCORE)
    if key not in _NC_CACHE:
        _NC_CACHE[key] = build(cfg)
    nc = _NC_CACHE[key]
    maps = make_in_maps(cfg, inputs)
    res = run_bass_kernel_spmd(nc, maps, core_ids=list(range(cfg.NCORE)))
    outs = [r["outT"] for r in res.results]
    full = np.concatenate([o.T for o in outs], axis=0)
    return np.ascontiguousarray(full.reshape(cfg.B, cfg.S, cfg.D).astype(np.float32))


def kernel(**inputs):
    return run(Cfg(), inputs)
```

```python
import numpy as np
from contextlib import ExitStack
import concourse.bass as bass
import concourse.mybir as mybir
from concourse.bass_utils import run_bass_kernel_spmd

F32 = mybir.dt.float32
BF16 = mybir.dt.bfloat16
AF = mybir.ActivationFunctionType
ALU = mybir.AluOpType
AX = mybir.AxisListType

NR = 8
HD = 128


class Cfg:
    def __init__(self, D=4096, S=2048, B=4, E=64, L=2, F=256, NCORE=4):
        self.D, self.S, self.B, self.E, self.L, self.F, self.NCORE = D, S, B, E, L, F, NCORE
        self.KC = D // 128
        self.BL = B // NCORE
        self.NTL = self.BL * S
        self.NCH = S // 128
        self.NG = E // 8
        assert S % 512 == 0 and E % 8 == 0 and B % NCORE == 0


class Buf:
    __slots__ = ("name", "writer", "readers", "sem")

    def __init__(self, name):
        self.name = name
        self.writer = None
        self.readers = {}
        self.sem = None


class SemSlot:
    def __init__(self, sem):
        self.sem = sem
        self.count = 0
        self.bg = False


class Prog:
    ENGS = ("pe", "act", "dve", "pool", "sp")

    def __init__(self, nc, es):
        self.nc, self.es = nc, es
        self.q = {e: [] for e in self.ENGS}
        self.esem = {e: es.enter_context(nc.semaphore("S_" + e)) for e in self.ENGS}
        self.allbufs = []
        self.active = []
        self.free = []
        self.nslots = 0
        self.bgbufs = set()

    def buf(self, name=None):
        b = Buf(f"{name or 'b'}{len(self.allbufs)}")
        self.allbufs.append(b)
        return b

    def bufs(self, n, name="b"):
        return [self.buf(name) for _ in range(n)]

    def op(self, eng, fn, reads=(), writes=()):
        deps = []
        for b in reads:
            if b.writer is not None:
                deps.append(b.writer)
        for b in writes:
            w = b.writer
            if w is not None and not (w[0] == 'op' and w[1] == eng):
                deps.append(w)
            for r in b.readers.values():
                if not (r[0] == 'op' and r[1] == eng):
                    deps.append(r)
        if eng == "pe":
            deps = [d for d in deps if not (d[0] == 'op' and d[1] == 'pe')]
        idx = len(self.q[eng])
        self.q[eng].append({"fn": fn, "deps": deps, "needed": False, "dma": None})
        me = ('op', eng, idx)
        for b in reads:
            b.readers[eng] = me
        for b in writes:
            b.writer = me
            b.readers = {}
        return me

    def dma(self, eng, out, in_, reads=(), writes=(), sem_buf=None, slow=False, bg=False):
        sb = sem_buf or (writes[0] if writes else reads[0])
        if sb.sem is None:
            if self.free and not bg:
                sb.sem = self.free.pop()
            else:
                self.nslots += 1
                sb.sem = SemSlot(self.es.enter_context(self.nc.semaphore(f"D{self.nslots}")))
            if bg:
                sb.sem.bg = True
                self.bgbufs.add(id(sb))
            self.active.append(sb)
        slot = sb.sem
        deps = []
        for b in reads:
            if b.writer is not None:
                deps.append(b.writer)
        for b in writes:
            if b.writer is not None:
                deps.append(b.writer)
            deps.extend(b.readers.values())
        slot.count += 1
        me = ('dma', slot, slot.count)
        if slow:
            fn = (lambda e, o=out, i=in_: e.dma_start(out=o, in_=i, allow_slow_non_contiguous=True))
        else:
            fn = (lambda e, o=out, i=in_: e.dma_start(out=o, in_=i))
        self.q[eng].append({"fn": fn, "deps": deps, "needed": False, "dma": slot})
        for b in reads:
            b.readers[('dma', id(slot))] = me
        for b in writes:
            b.writer = me
            b.readers = {}
        return me

    def barrier(self, final=False):
        deps = []
        for e in self.ENGS:
            for i in range(len(self.q[e]) - 1, -1, -1):
                it = self.q[e][i]
                if it["dma"] is None and it["fn"] is not None:
                    deps.append(('op', e, i))
                    break
        for b in self.active:
            if final or not b.sem.bg:
                deps.append(('dma', b.sem, b.sem.count))
        for e in self.ENGS:
            self.q[e].append({"fn": None, "deps": list(deps), "needed": False, "dma": None})
        keep = []
        for b in self.active:
            if b.sem.bg:
                keep.append(b)
            else:
                self.free.append(b.sem)
                b.sem = None
        self.active = keep
        for b in self.allbufs:
            if id(b) in self.bgbufs:
                continue
            b.writer = None
            b.readers = {}

    def emit(self):
        nc = self.nc
        for e in self.ENGS:
            for it in self.q[e]:
                for d in it["deps"]:
                    if d[0] == 'op':
                        self.q[d[1]][d[2]]["needed"] = True
        cnt = {}
        for e in self.ENGS:
            c = 0
            arr = []
            for it in self.q[e]:
                if it["needed"]:
                    c += 1
                arr.append(c)
            cnt[e] = arr
        self.stats = {e: (len(self.q[e]), cnt[e][-1] if cnt[e] else 0) for e in self.ENGS}
        self.stats["slots"] = self.nslots

        def replay(ename, eng):
            waited = {}
            for i, it in enumerate(self.q[ename]):
                for d in it["deps"]:
                    if d[0] == 'op':
                        if d[1] == ename and d[2] >= i:
                            continue
                        sem = self.esem[d[1]]
                        val = cnt[d[1]][d[2]]
                    else:
                        sem = d[1].sem
                        val = 16 * d[2]
                    key = id(sem)
                    if waited.get(key, 0) >= val:
                        continue
                    waited[key] = val
                    eng.wait_ge(sem, val)
                if it["fn"] is None:
                    continue
                ins = it["fn"](eng)
                if it["dma"] is not None:
                    ins.then_inc(it["dma"].sem, 16)
                elif it["needed"]:
                    ins.then_inc(self.esem[ename], 1)

        with nc.Block() as block:
            @block.tensor
            def _(e):
                replay("pe", e)

            @block.scalar
            def _(e):
                replay("act", e)

            @block.vector
            def _(e):
                replay("dve", e)

            @block.gpsimd
            def _(e):
                replay("pool", e)

            @block.sync
            def _(e):
                replay("sp", e)


class Arena:
    def __init__(self, nc, es, words, name="arena"):
        self.t = es.enter_context(nc.sbuf_tensor(name, [128, words], F32))
        self.words = words
        self.off = 0

    def mark(self):
        return self.off

    def release(self, m):
        self.off = m

    def alloc(self, shape, dtype):
        n = int(np.prod(shape[1:]))
        w = n if dtype == F32 else (n + 1) // 2
        w = (w + 7) // 8 * 8
        assert self.off + w <= self.words, f"arena overflow {self.off}+{w}>{self.words}"
        ap = self.t[0:shape[0], self.off:self.off + w]
        self.off += w
        if dtype != F32:
            ap = ap.bitcast(dtype)
        ap = ap[:, 0:n]
        if len(shape) > 2:
            names = " ".join(f"d{i}" for i in range(1, len(shape)))
            kw = {f"d{i}": shape[i] for i in range(2, len(shape))}
            ap = ap.rearrange(f"p ({names}) -> p {names}", **kw)
        return ap


def build(cfg):
    D, S, B, E, L, KC, BL, NTL, NCH, NG = (cfg.D, cfg.S, cfg.B, cfg.E, cfg.L, cfg.KC, cfg.BL, cfg.NTL,
                                            cfg.NCH, cfg.NG)
    NBK = NTL // 512
    BPB = S // 512
    NCL = BL * NCH
    nc = bass.Bass("TRN2", target_bir_lowering=False)

    def din(name, shape, dt=F32):
        return nc.dram_tensor(name, list(shape), dt, kind="ExternalInput").ap()

    def dint(name, shape, dt=F32):
        return nc.dram_tensor(name, list(shape), dt)

    I = {}
    I["xT"] = din("xT", [D, NTL])
    I["cT"] = din("cT", [128, KC * B])
    I["selb"] = din("selb", [128, BL * B])
    I["wada"] = din("wada", [D, 6 * D])
    I["bada"] = din("bada", [128, 6 * KC])
    I["tab"] = din("tab", [128, L * 6 * KC])
    I["ng"] = din("ng", [128, L * 2 * KC])
    I["win"] = din("win", [L, 8, D, 1409])
    I["wout"] = din("wout", [L, 4096, D])
    I["lrup"] = din("lrup", [128, L * 8 * 8])
    I["lruw"] = din("lruw", [L, 8, 2, 128, 128])
    I["sgbc"] = din("sgbc", [128, L * 8 * 2 * 128])
    I["sgwT"] = din("sgwT", [L, 8, 128, 128])
    I["foxp"] = din("foxp", [128, L * 8 * 3])
    I["rw"] = din("rw", [L, D, E])
    I["rbbc"] = din("rbbc", [128, L * E])
    I["ewg"] = din("ewg", [L, E, 2, 128, KC * 128])
    I["ewu"] = din("ewu", [L, E, 2, 128, KC * 128])
    I["ewd"] = din("ewd", [L, E, 256, D])
    I["swg"] = din("swg", [L, D, 256])
    I["swu"] = din("swu", [L, D, 256])
    I["swd"] = din("swd", [L, 256, D])
    I["selB"] = din("selB", [NG, E, 8 * 128])
    I["cst"] = din("cst", [128, 4 * 128])
    I["hcst"] = din("hcst", [8, 128, 2 * 128 + 2])
    I["rot"] = din("rot", [128, 2 * S])
    outT = nc.dram_tensor("outT", [D, NTL], F32, kind="ExternalOutput").ap()

    hT = dint("hT", [D, NTL], BF16)
    PF = dint("PF", [8, 8 * 128, NTL], F32)
    PT = dint("PT", [8, NTL, 384], F32)
    PTf = dint("PTf", [8, 128, NTL // 128], F32)
    yT = dint("yT", [4096, NTL], BF16)
    xres = dint("xres", [D, NTL], F32)
    shr = dint("shr", [D, NTL], F32)
    gTd = dint("gTd", [E, NTL], F32)
    wgb = dint("wgb", [E, 2, 128, KC * 128], BF16)
    wub = dint("wub", [E, 2, 128, KC * 128], BF16)
    DG = D // 256
    wdb = dint("wdb", [NG, DG, 128, 16 * 256], BF16)

    es = ExitStack()
    P = Prog(nc, es)
    A = Arena(nc, es, 52224)
    ps = [es.enter_context(nc.psum_tensor(f"ps{i}", [128, 512], F32)) for i in range(8)]
    psb = P.bufs(8, "ps")
    psi = [0]

    psmode = [6]

    def nextps():
        k = psi[0] % psmode[0]
        psi[0] += 1
        return ps[k], psb[k]

    def accps(i):
        return ps[psmode[0] + i], psb[psmode[0] + i]

    def run_gens(gens):
        gens = list(gens)
        while gens:
            for g_ in list(gens):
                try:
                    next(g_)
                except StopIteration:
                    gens.remove(g_)

    db = {n: P.buf(n) for n in ["hT", "PF", "PT", "PTf", "yT", "xres", "shr", "gTd", "wgb", "wub", "wdb", "outT"]}
    NB = P.buf("never")

    def mm(out, lhsT, rhs, start, stop, reads, writes):
        return P.op("pe", lambda e: e.matmul(out, lhsT, rhs, start=start, stop=stop), reads, writes)

    def tr(out, in_, ident, reads, writes):
        return P.op("pe", lambda e: e.transpose(out, in_, ident), reads, writes)

    def act(out, in_, func, reads, writes, bias=None, scale=None, accum=None):
        kw = {}
        if bias is not None:
            kw["bias"] = bias
        if scale is not None:
            kw["scale"] = scale
        if accum is not None:
            kw["accum_out"] = accum
        return P.op("act", lambda e: e.activation(out=out, in_=in_, func=func, **kw), reads, writes)

    def tt(eng, out, in0, in1, op, reads, writes):
        return P.op(eng, lambda e: e.tensor_tensor(out=out, in0=in0, in1=in1, op=op), reads, writes)

    def ts(eng, out, in0, s1, s2, op0, op1, reads, writes):
        if op1 is None:
            return P.op(eng, lambda e: e.tensor_scalar(out=out, in0=in0, scalar1=s1, scalar2=None, op0=op0),
                        reads, writes)
        return P.op(eng, lambda e: e.tensor_scalar(out=out, in0=in0, scalar1=s1, scalar2=s2, op0=op0, op1=op1),
                    reads, writes)

    def stt(out, in0, scalar, in1, op0, op1, reads, writes):
        return P.op("dve", lambda e: e.scalar_tensor_tensor(out=out, in0=in0, scalar=scalar, in1=in1,
                                                            op0=op0, op1=op1), reads, writes)

    def cp(eng, out, in_, reads, writes):
        if eng == "act":
            return P.op("act", lambda e: e.copy(out=out, in_=in_), reads, writes)
        return P.op(eng, lambda e: e.tensor_copy(out=out, in_=in_), reads, writes)

    def recip(out, in_, reads, writes):
        return P.op("dve", lambda e: e.reciprocal(out=out, in_=in_), reads, writes)

    cst = A.alloc([128, 4 * 128], F32)
    b_cst = P.buf("cst")
    P.dma("sp", cst, I["cst"], writes=[b_cst])
    ident_f = cst[:, 0:128]
    triu_f = cst[:, 128:256]
    ones_f = cst[:, 256:384]
    Pm_f = cst[:, 384:512]
    cstb = A.alloc([128, 3 * 128], BF16)
    b_cstb = P.buf("cstb")
    cp("dve", cstb, cst[:, 0:384], [b_cst], [b_cstb])
    ident_b = cstb[:, 0:128]
    triu_b = cstb[:, 128:256]
    ones_b = cstb[:, 256:384]
    epsv = A.alloc([128, 2], F32)
    b_eps = P.buf("eps")
    P.op("dve", lambda e: e.memset(epsv[:, 0:1], 1e-6), [], [b_eps])
    P.op("dve", lambda e: e.memset(epsv[:, 1:2], 1.0), [], [b_eps])

    def eps_ap(v):
        return epsv[:, 0:1] if v < 0.5 else epsv[:, 1:2]

    modv = A.alloc([128, BL * L * 6, KC], F32)
    b_modv = P.buf("modv")

    def MV(bl, l, i):
        return modv[:, (bl * L + l) * 6 + i, :]
    srs = A.alloc([128, NCL], F32)
    b_srs = P.buf("srs")

    def phase_mod():
        m0 = A.mark()
        ct = A.alloc([128, KC, B], F32)
        cs = A.alloc([128, KC, B], BF16)
        sg = A.alloc([128, KC, B], F32)
        b_ct, b_cs, b_sg = P.buf("ct"), P.buf("cs"), P.buf("sg")
        P.dma("sp", ct, I["cT"].rearrange("p (k b) -> p k b", b=B), writes=[b_ct])
        act(sg, ct, AF.Sigmoid, [b_ct], [b_sg])
        tt("dve", cs, ct, sg, ALU.mult, [b_ct, b_sg], [b_cs])
        wb = [A.alloc([128, KC, 128], BF16) for _ in range(3)]
        b_wb = P.bufs(3, "wada")
        ma = A.alloc([128, 6 * KC, B], F32)
        b_ma = P.buf("ma")
        wsrc = I["wada"].rearrange("(k p) c -> p k c", p=128)
        NJ = 6 * KC
        JB = 512 // B
        for j in range(NJ):
            w = j % 3
            P.dma("pool", wb[w], wsrc[:, :, j * 128:(j + 1) * 128], writes=[b_wb[w]])
            pt, pb = accps((j // JB) % 2)
            jj = j % JB
            for kc in range(KC):
                mm(pt[:, jj * B:(jj + 1) * B], wb[w][:, kc, :], cs[:, kc, :], kc == 0, kc == KC - 1,
                   [b_wb[w], b_cs], [pb])
            if jj == JB - 1 or j == NJ - 1:
                j0 = (j // JB) * JB
                cp("dve", ma[:, j0:j + 1, :].rearrange("p j b -> p (j b)"), pt[:, 0:(j + 1 - j0) * B], [pb], [b_ma])
        selb = A.alloc([128, BL, B], F32)
        bada = A.alloc([128, 6 * KC], F32)
        tab = A.alloc([128, L, 6 * KC], F32)
        ng = A.alloc([128, L, 2, KC], F32)
        b_small = P.buf("small")
        P.dma("sp", selb, I["selb"].rearrange("p (a b) -> p a b", b=B), writes=[b_small])
        P.dma("sp", bada, I["bada"], writes=[b_small])
        P.dma("sp", tab, I["tab"].rearrange("p (l x) -> p l x", l=L), writes=[b_small])
        P.dma("sp", ng, I["ng"].rearrange("p (l t k) -> p l t k", l=L, t=2), writes=[b_small])
        mm_ = A.alloc([128, 6 * KC], F32)
        ml = A.alloc([128, 6, KC], F32)
        b_mm, b_ml = P.buf("modm"), P.buf("ml")
        for bl in range(BL):
            ts("dve", mm_, ma[:, :, 0], selb[:, bl, 0:1], None, ALU.mult, None, [b_ma, b_small], [b_mm])
            for b in range(1, B):
                stt(mm_, ma[:, :, b], selb[:, bl, b:b + 1], mm_, ALU.mult, ALU.add, [b_ma, b_small, b_mm], [b_mm])
            tt("dve", mm_, mm_, bada, ALU.add, [b_mm, b_small], [b_mm])
            for l in range(L):
                tt("dve", ml.rearrange("p a k -> p (a k)"), mm_, tab[:, l, :], ALU.add, [b_mm, b_small], [b_ml])
                for h in range(2):
                    sh, sc, gt = ml[:, 3 * h + 0, :], ml[:, 3 * h + 1, :], ml[:, 3 * h + 2, :]
                    stt(MV(bl, l, 3 * h + 0), sc, 1.0, ng[:, l, h, :], ALU.add, ALU.mult, [b_ml, b_small], [b_modv])
                    cp("dve", MV(bl, l, 3 * h + 1), sh, [b_ml], [b_modv])
                    cp("dve", MV(bl, l, 3 * h + 2), gt, [b_ml], [b_modv])
        P.barrier()
        A.release(m0)

    def phase_norm(l, h, src_ap, src_buf, extra=None):
        m0 = A.mark()
        xb = A.alloc([128, KC, 512], F32)
        hb = A.alloc([128, KC, 512], BF16)
        sq = [A.alloc([128, 512], BF16) for _ in range(3)]
        rs = A.alloc([128, 512], F32)
        rt = A.alloc([128, 512], F32)
        b_x, b_h, b_rs, b_rt = P.buf("x"), P.buf("h"), P.buf("rs"), P.buf("rt")
        b_sq = P.bufs(3, "sq")
        if extra:
            extra("alloc", None)
        srcv = src_ap.rearrange("(k p) t -> p k t", p=128)
        dstv = hT.ap().rearrange("(k p) t -> p k t", p=128)
        for tb in range(NBK):
            bl = tb // BPB
            tsl = slice(tb * 512, (tb + 1) * 512)
            P.dma("sp", xb, srcv[:, :, tsl], reads=[src_buf], writes=[b_x])
            pt, pb = nextps()
            for kc in range(KC):
                i = kc % 3
                act(sq[i], xb[:, kc, :], AF.Square, [b_x], [b_sq[i]])
                mm(pt[:, :], ones_b, sq[i], kc == 0, kc == KC - 1, [b_sq[i], b_cstb], [pb])
            act(rt, pt[:, :], AF.Sqrt, [pb, b_eps], [b_rt], bias=eps_ap(1e-6), scale=1.0 / D)
            recip(rs, rt, [b_rt], [b_rs])
            tt("dve", xb, xb, rs.unsqueeze(1).to_broadcast([128, KC, 512]), ALU.mult, [b_x, b_rs], [b_x])
            for kc in range(KC):
                act(hb[:, kc, :], xb[:, kc, :], AF.Identity, [b_x, b_modv], [b_h],
                    bias=MV(bl, l, 3 * h + 1)[:, kc:kc + 1], scale=MV(bl, l, 3 * h + 0)[:, kc:kc + 1])
            P.dma("sp", dstv[:, :, tsl], hb, reads=[b_h], writes=[db["hT"]])
            if extra:
                extra("post", (tb, bl, hb, b_h))
        P.barrier()
        A.release(m0)

    def phase_inproj_all(l):
        m0 = A.mark()
        wqA = A.alloc([128, KC, 1024], BF16)
        wqB = A.alloc([128, KC, 385], BF16)
        b_wA, b_wB = P.buf("wqA"), P.buf("wqB")
        hb = [A.alloc([128, KC, 512], BF16) for _ in range(2)]
        b_hb = P.bufs(2, "hblk")
        sf = [A.alloc([128, 512], F32) for _ in range(4)]
        b_sf = P.bufs(4, "sf")
        st = [A.alloc([128, 385], F32) for _ in range(2)]
        b_st = P.bufs(2, "st")
        hv = hT.ap().rearrange("(k p) t -> p k t", p=128)
        step = max(1, KC // 8)

        def loadW(c, dst, bdst, c0, c1):
            wsrc = I["win"][l, c].rearrange("(k p) c -> p k c", p=128)
            for k0 in range(0, KC, step):
                P.dma("pool", dst[:, k0:k0 + step, :], wsrc[:, k0:k0 + step, c0:c1], writes=[bdst])
        steps = [(c, part, tb) for c in range(8) for part in range(2) for tb in range(NBK)]

        def loadH(i):
            tb = steps[i][2]
            P.dma("sp", hb[i % 2], hv[:, :, tb * 512:(tb + 1) * 512], reads=[db["hT"]], writes=[b_hb[i % 2]])
        loadW(0, wqA, b_wA, 0, 1024)
        loadW(0, wqB, b_wB, 1024, 1409)
        loadH(0)
        ci = 0
        for i, (c, part, tb) in enumerate(steps):
            if i + 1 < len(steps):
                loadH(i + 1)
            hk = hb[i % 2]
            bh = b_hb[i % 2]
            if part == 0:
                for cb in range(8):
                    pt, pb = nextps()
                    for kc in range(KC):
                        mm(pt[:, :], wqA[:, kc, cb * 128:(cb + 1) * 128], hk[:, kc, :], kc == 0, kc == KC - 1,
                           [b_wA, bh], [pb])
                    j = ci % 4
                    ci += 1
                    cp("act" if ci % 2 else "dve", sf[j], pt[:, :], [pb], [b_sf[j]])
                    P.dma("sp", PF[c, cb * 128:(cb + 1) * 128, tb * 512:(tb + 1) * 512], sf[j], reads=[b_sf[j]],
                          writes=[db["PF"]])
                if tb == NBK - 1 and c + 1 < 8:
                    loadW(c + 1, wqA, b_wA, 0, 1024)
            else:
                for t4 in range(4):
                    pt, pb = nextps()
                    for kc in range(KC):
                        mm(pt[:, 0:385], hk[:, kc, t4 * 128:(t4 + 1) * 128], wqB[:, kc, :], kc == 0,
                           kc == KC - 1, [b_wB, bh], [pb])
                    j = t4 % 2
                    cp("act" if t4 % 2 else "dve", st[j], pt[:, 0:385], [pb], [b_st[j]])
                    r0 = tb * 512 + t4 * 128
                    P.dma("sp", PT[c, r0:r0 + 128, :], st[j][:, 0:384], reads=[b_st[j]], writes=[db["PT"]])
                    P.dma("sp", PTf[c, :, r0 // 128:r0 // 128 + 1], st[j][:, 384:385], reads=[b_st[j]],
                          writes=[db["PTf"]], slow=True)
                if tb == NBK - 1 and c + 1 < 8:
                    loadW(c + 1, wqB, b_wB, 1024, 1409)
        P.barrier()
        A.release(m0)

    def gelu(eng, out, x, t1, rx, wout_b, b_t1):
        tt(eng, t1, x, x, ALU.mult, rx, [b_t1])
        ts(eng, t1, t1, 0.044715, 1.0, ALU.mult, ALU.add, [b_t1], [b_t1])
        tt(eng, t1, t1, x, ALU.mult, rx + [b_t1], [b_t1])
        act(t1, t1, AF.Sigmoid, [b_t1], [b_t1], scale=1.5957691216)
        tt(eng, out, t1, x, ALU.mult, rx + [b_t1], [wout_b])

    def phase_sgu_stats(l):
        m0 = A.mark()
        junk = A.alloc([128, 128], F32)
        ssq = A.alloc([128, 8, NCL], F32)
        srt = A.alloc([128, NCL], F32)
        b_ssq, b_srt = P.buf("ssq"), P.buf("srt")
        pend = []

        def gen(c):
            gv = A.alloc([128, NCH, 128], F32)
            t1 = A.alloc([128, NCH, 128], F32)
            jk = A.alloc([128, 128], F32)
            b_gv, b_t1, b_junk, b_sq1 = P.buf("gv"), P.buf("t1"), P.buf("junk"), P.buf("sq1")
            for bl in range(BL):
                src = PT[c, bl * S:(bl + 1) * S, 128:256].rearrange("(n m) e -> m n e", m=128)
                P.dma("sp", gv, src, reads=[db["PT"]], writes=[b_gv])
                yield
                gelu("dve", gv, gv, t1, [b_gv], b_gv, b_t1)
                yield
                for n in range(NCH):
                    act(jk, gv[:, n, :], AF.Square, [b_gv], [b_junk, b_sq1],
                        accum=ssq[:, c, bl * NCH + n:bl * NCH + n + 1])
                P.dma("sp", src, gv, reads=[b_gv], writes=[db["PT"]])
                yield
            pend.append(b_sq1)
        for c0 in range(0, 8, 2):
            m1 = A.mark()
            del pend[:]
            wcast_slice(c0 // 2, 20)
            run_gens([gen(c0), gen(c0 + 1)])
            if c0 == 6:
                P.op("dve", lambda e: e.tensor_reduce(out=srt, in_=ssq.rearrange("p c x -> p x c"), axis=AX.X,
                                                      op=ALU.add), list(pend), [b_srt])
                act(srt, srt, AF.Sqrt, [b_srt, b_eps], [b_srt], bias=eps_ap(1e-6), scale=1.0 / 1024)
                recip(srs, srt, [b_srt], [b_srs])
            P.barrier()
            A.release(m1)
        A.release(m0)

    def load_head(l, c, rot, b_rot):
        hc = A.alloc([128, 258], F32)
        lrup = A.alloc([128, 8], F32)
        lw = A.alloc([128, 2, 128], BF16)
        sgbc = A.alloc([128, 2, 128], F32)
        sgw = A.alloc([128, 128], F32)
        sgwb = A.alloc([128, 128], BF16)
        foxp = A.alloc([128, 3], F32)
        b_pp, b_sgwb = P.buf("pp"), P.buf("sgwb")
        P.dma("sp", hc, I["hcst"][c], writes=[b_pp])
        o8 = (l * 8 + c) * 8
        P.dma("sp", lrup, I["lrup"][:, o8:o8 + 8], writes=[b_pp])
        lwf = A.alloc([128, 2, 128], F32)
        b_lwf = P.buf("lwf")
        P.dma("sp", lwf, I["lruw"][l, c].rearrange("t i j -> i t j"), writes=[b_lwf])
        b_lw = P.buf("lw")
        cp("dve", lw, lwf, [b_lwf], [b_lw])
        o2 = (l * 8 + c) * 256
        P.dma("sp", sgbc, I["sgbc"][:, o2:o2 + 256].rearrange("p (t x) -> p t x", t=2), writes=[b_pp])
        P.dma("sp", sgw, I["sgwT"][l, c], writes=[b_pp])
        o3 = (l * 8 + c) * 3
        P.dma("sp", foxp, I["foxp"][:, o3:o3 + 3], writes=[b_pp])
        decT_f, qdbc_f, kd_f, cd_f = hc[:, 0:128], hc[:, 128:256], hc[:, 256:257], hc[:, 257:258]
        tt("dve", sgwb, sgw, triu_f, ALU.mult, [b_pp, b_cst], [b_sgwb])
        m8 = A.alloc([128, 2], F32)
        b_m8 = P.buf("m8")
        act(m8[:, 0:1], lrup[:, 7:8], AF.Exp, [b_pp], [b_m8], scale=-1.0)
        act(m8[:, 0:1], m8[:, 0:1], AF.Ln, [b_m8, b_eps], [b_m8], bias=eps_ap(1.0))
        ts("dve", m8[:, 1:2], m8[:, 0:1], -8.0, None, ALU.mult, None, [b_m8], [b_m8])
        nfb = A.alloc([128, 1], F32)
        b_nfb = P.buf("nfb")
        ts("dve", nfb, foxp[:, 2:3], -1.0, None, ALU.mult, None, [b_pp], [b_nfb])
        return dict(locals())

    def g_sgu(l, c, hp, hi):
        lrup, lw, sgbc, sgwb, foxp, m8, nfb = [hp[k] for k in ["lrup", "lw", "sgbc", "sgwb", "foxp", "m8", "nfb"]]
        b_pp, b_sgwb, b_m8, b_nfb = hp["b_pp"], hp["b_sgwb"], hp["b_m8"], hp["b_nfb"]
        yv = yT.ap()
        PFv = PF[c]
        PTv = PT[c]

        def yrow(mix):
            r0 = mix * 1024 + c * 128
            return slice(r0, r0 + 128)
        gv = A.alloc([128, NCH, 128], F32)
        vb = A.alloc([128, NCH, 128], BF16)
        uT = A.alloc([128, S], F32)
        ut1 = A.alloc([128, S], F32)
        yo = A.alloc([128, S], BF16)
        b_gv, b_vb, b_uT, b_ut1, b_yo = P.buf("gv"), P.buf("vb"), P.buf("uT"), P.buf("ut1"), P.buf("yo")
        for bl in range(BL):
            bs = slice(bl * S, (bl + 1) * S)
            sl = slice(bl * NCH, (bl + 1) * NCH)
            P.dma("sp", gv, PTv[bs, 128:256].rearrange("(n m) e -> m n e", m=128), reads=[db["PT"]], writes=[b_gv])
            tt("dve", gv, gv, srs[:, sl].unsqueeze(2).to_broadcast([128, NCH, 128]), ALU.mult, [b_gv, b_srs], [b_gv])
            tt("dve", vb, gv, sgbc[:, 0, :].unsqueeze(1).to_broadcast([128, NCH, 128]), ALU.mult, [b_gv, b_pp], [b_vb])
            P.dma("sp", uT, PFv[5 * 128:6 * 128, bs], reads=[db["PF"]], writes=[b_uT])
            gelu("dve", uT, uT, ut1, [b_uT], b_uT, b_ut1)
            for q4 in range(NCH // 4):
                pt, pb = nextps()
                for i in range(4):
                    n = q4 * 4 + i
                    mm(pt[:, i * 128:(i + 1) * 128], vb[:, n, :], sgwb, True, True, [b_vb, b_sgwb], [pb])
                tsl = slice(q4 * 512, (q4 + 1) * 512)
                tt("dve", ut1[:, tsl].rearrange("p (a t) -> p a t", a=4), pt[:, :].rearrange("p (a t) -> p a t", a=4),
                   sgbc[:, 1, :].unsqueeze(1).to_broadcast([128, 4, 128]), ALU.add, [pb, b_pp, b_ut1], [b_ut1])
                tt("dve", yo[:, tsl], ut1[:, tsl], uT[:, tsl], ALU.mult, [b_ut1, b_uT], [b_yo])
                yield
            P.dma("sp", yv[yrow(2), bs], yo, reads=[b_yo], writes=[db["yT"]])
            yield

    def g_lru(l, c, hp, hi):
        lrup, lw, sgbc, sgwb, foxp, m8, nfb = [hp[k] for k in ["lrup", "lw", "sgbc", "sgwb", "foxp", "m8", "nfb"]]
        b_pp, b_sgwb, b_m8, b_nfb = hp["b_pp"], hp["b_sgwb"], hp["b_m8"], hp["b_nfb"]
        yv = yT.ap()
        PFv = PF[c]
        PTv = PT[c]

        def yrow(mix):
            r0 = mix * 1024 + c * 128
            return slice(r0, r0 + 128)
        x = A.alloc([128, S], F32)
        xc = A.alloc([128, S], F32)
        xcb = A.alloc([128, S], BF16)
        rg = A.alloc([128, S], F32)
        ig = A.alloc([128, S], F32)
        gt = A.alloc([128, S], F32)
        gt1 = A.alloc([128, S], F32)
        yo = A.alloc([128, S], BF16)
        b_x, b_xc, b_xcb, b_rg, b_ig, b_gt, b_gt1, b_yo = [P.buf(n) for n in
                                                            ["x", "xc", "xcb", "rg", "ig", "gt", "gt1", "yo"]]
        for bl in range(BL):
            bs = slice(bl * S, (bl + 1) * S)
            P.dma("sp", x, PFv[4 * 128:5 * 128, bs], reads=[db["PF"]], writes=[b_x])
            P.dma("sp", gt, PFv[3 * 128:4 * 128, bs], reads=[db["PF"]], writes=[b_gt])
            ts("dve", xc, x, lrup[:, 3:4], lrup[:, 4:5], ALU.mult, ALU.add, [b_x, b_pp], [b_xc])
            for j in range(3):
                sh = 3 - j
                stt(xc[:, sh:S], x[:, 0:S - sh], lrup[:, j:j + 1], xc[:, sh:S], ALU.mult, ALU.add,
                    [b_x, b_pp, b_xc], [b_xc])
            cp("act", xcb, xc, [b_xc], [b_xcb])
            yield
            for q in range(S // 512):
                tsl = slice(q * 512, (q + 1) * 512)
                pt, pb = nextps()
                mm(pt[:, :], lw[:, 0, :], xcb[:, tsl], True, True, [hp["b_lw"], b_xcb], [pb])
                act(rg[:, tsl], pt[:, :], AF.Sigmoid, [pb, b_pp], [b_rg], bias=lrup[:, 5:6])
                pt2, pb2 = nextps()
                mm(pt2[:, :], lw[:, 1, :], xcb[:, tsl], True, True, [hp["b_lw"], b_xcb], [pb2])
                act(ig[:, tsl], pt2[:, :], AF.Sigmoid, [pb2, b_pp], [b_ig], bias=lrup[:, 6:7])
                yield
            act(rg, rg, AF.Exp, [b_rg, b_m8], [b_rg], scale=m8[:, 1:2])
            tt("dve", ig, ig, xc, ALU.mult, [b_ig, b_xc], [b_ig])
            yield
            tt("dve", x, rg, rg, ALU.mult, [b_rg], [b_x])
            ts("dve", x, x, -1.0, 1.0, ALU.mult, ALU.add, [b_x], [b_x])
            ts("dve", x, x, 0.0, None, ALU.max, None, [b_x], [b_x])
            act(x, x, AF.Sqrt, [b_x], [b_x])
            yield
            tt("dve", ig, ig, x, ALU.mult, [b_ig, b_x], [b_ig])
            P.op("dve", lambda e: e.tensor_tensor_scan(out=xc, data0=rg, data1=ig, initial=0.0, op0=ALU.mult,
                                                       op1=ALU.add), [b_rg, b_ig], [b_xc])
            gelu("dve", gt, gt, gt1, [b_gt], b_gt, b_gt1)
            tt("dve", yo, xc, gt, ALU.mult, [b_xc, b_gt], [b_yo])
            P.dma("sp", yv[yrow(1), bs], yo, reads=[b_yo], writes=[db["yT"]])
            yield

    def g_ret(l, c, hp, hi):
        rot, b_rot = hp["rot"], hp["b_rot"]
        decT_f, qdbc_f, kd_f, cd_f = hp["decT_f"], hp["qdbc_f"], hp["kd_f"], hp["cd_f"]
        b_pp = hp["b_pp"]
        yv = yT.ap()
        PFv = PF[c]
        PTv = PT[c]

        def yrow(mix):
            r0 = mix * 1024 + c * 128
            return slice(r0, r0 + 128)
        q = A.alloc([128, S], F32)
        kk = A.alloc([128, S], F32)
        tmp = A.alloc([128, S], F32)
        qr = A.alloc([128, S], BF16)
        kr = A.alloc([128, S], BF16)
        qs = A.alloc([128, S], BF16)
        vtm = A.alloc([128, NCH, 128], BF16)
        vst = A.alloc([128, NCH, 128], F32)
        b_vst = P.buf("vst")
        g = A.alloc([128, S], F32)
        yr = A.alloc([128, S], F32)
        ysq = A.alloc([128, 512], F32)
        yo = A.alloc([128, S], BF16)
        prev = A.alloc([128, 128], F32)
        prevb = A.alloc([128, 128], BF16)
        sT = [A.alloc([128, 128], BF16) for _ in range(2)]
        kd = [A.alloc([128, 128], BF16) for _ in range(2)]
        rsr = A.alloc([128, 512], F32)
        (b_q, b_k, b_tmp, b_qr, b_kr, b_qs, b_v, b_g, b_yr, b_ysq, b_yo, b_prev, b_prevb, b_rsr) = [
            P.buf(n) for n in ["q", "k", "tmp", "qr", "kr", "qs", "v", "g", "yr", "ysq", "yo", "prev", "prevb",
                               "rsr"]]
        b_sT = P.bufs(2, "sT")
        b_kd = P.bufs(2, "kd")
        cosT, sinT = rot[:, 0, :], rot[:, 1, :]

        def rotary(src, dst_b, bsrc, bdst, scale):
            for qq in range(S // 512):
                tsl = slice(qq * 512, (qq + 1) * 512)
                pt, pb = nextps()
                mm(pt[:, :], Pm_f, src[:, tsl], True, True, [b_cst, bsrc], [pb])
                tt("dve", tmp[:, tsl], pt[:, :], sinT[:, tsl], ALU.mult, [pb, b_rot], [b_tmp])
            tt("dve", src, src, cosT, ALU.mult, [bsrc, b_rot], [bsrc])
            if scale == 1.0:
                tt("dve", dst_b, src, tmp, ALU.add, [bsrc, b_tmp], [bdst])
            else:
                tt("dve", src, src, tmp, ALU.add, [bsrc, b_tmp], [bsrc])
                ts("dve", dst_b, src, scale, None, ALU.mult, None, [bsrc], [bdst])

        for bl in range(BL):
            bs = slice(bl * S, (bl + 1) * S)
            P.dma("sp", q, PFv[0:128, bs], reads=[db["PF"]], writes=[b_q])
            P.dma("sp", kk, PFv[128:256, bs], reads=[db["PF"]], writes=[b_k])
            P.dma("sp", g, PFv[256:384, bs], reads=[db["PF"]], writes=[b_g])
            P.dma("sp", vst, PTv[bs, 0:128].rearrange("(n m) e -> m n e", m=128), reads=[db["PT"]], writes=[b_vst])
            cp("act", vtm, vst, [b_vst], [b_v])
            rotary(q, qr, b_q, b_qr, 1.0)
            yield
            rotary(kk, kr, b_k, b_kr, float(HD ** -0.5))
            yield
            tt("dve", qs.rearrange("p (n c) -> p n c", c=128), qr.rearrange("p (n c) -> p n c", c=128),
               qdbc_f.unsqueeze(1).to_broadcast([128, NCH, 128]), ALU.mult, [b_qr, b_pp], [b_qs])
            P.op("dve", lambda e: e.memset(prev, 0.0), [], [b_prev])
            P.op("dve", lambda e: e.memset(prevb, 0.0), [], [b_prevb])
            act(g, g, AF.Silu, [b_g], [b_g])
            for q4 in range(NCH // 4):
                po, pob = accps(hi)
                for i in range(4):
                    n = q4 * 4 + i
                    csl = slice(n * 128, (n + 1) * 128)
                    j = n % 2
                    p1, p1b = nextps()
                    mm(p1[:, 0:128], kr[:, csl], qr[:, csl], True, True, [b_kr, b_qr], [p1b])
                    tt("dve", sT[j], p1[:, 0:128], decT_f, ALU.mult, [p1b, b_pp], [b_sT[j]])
                    p2, p2b = nextps()
                    p2v = p2[:, :].bitcast(BF16)
                    tr(p2v[:, 0:128], kr[:, csl], ident_b, [b_kr, b_cstb], [p2b])
                    ts("dve", kd[j], p2v[:, 0:128], kd_f, None, ALU.mult, None, [p2b, b_pp], [b_kd[j]])
                    mm(po[:, i * 128:(i + 1) * 128], vtm[:, n, :], sT[j], True, False, [b_v, b_sT[j]], [pob])
                    mm(po[:, i * 128:(i + 1) * 128], prevb, qs[:, csl], False, True, [b_prevb, b_qs], [pob])
                    p3, p3b = nextps()
                    mm(p3[:, 0:128], kd[j], vtm[:, n, :], True, True, [b_kd[j], b_v], [p3b])
                    stt(prev, prev, cd_f, p3[:, 0:128], ALU.mult, ALU.add, [b_prev, b_pp, p3b], [b_prev])
                    cp("act", prevb, prev, [b_prev], [b_prevb])
                    yield
                tsl = slice(q4 * 512, (q4 + 1) * 512)
                cp("act", yr[:, tsl], po[:, :], [pob], [b_yr])
                tt("dve", ysq, yr[:, tsl], yr[:, tsl], ALU.mult, [b_yr], [b_ysq])
                pn, pnb = nextps()
                mm(pn[:, :], ones_f, ysq, True, True, [b_cst, b_ysq], [pnb])
                act(rsr, pn[:, :], AF.Sqrt, [pnb, b_eps], [b_rsr], bias=eps_ap(1e-6), scale=1.0 / HD)
                recip(rsr, rsr, [b_rsr], [b_rsr])
                tt("dve", yr[:, tsl], yr[:, tsl], rsr, ALU.mult, [b_yr, b_rsr], [b_yr])
                tt("dve", yo[:, tsl], yr[:, tsl], g[:, tsl], ALU.mult, [b_yr, b_g], [b_yo])
                yield
            P.dma("sp", yv[yrow(0), bs], yo, reads=[b_yo], writes=[db["yT"]])
            yield

    def g_fox(l, c, hp, hi):
        foxp, nfb, b_pp, b_nfb = hp["foxp"], hp["nfb"], hp["b_pp"], hp["b_nfb"]
        yv = yT.ap()
        PFv = PF[c]
        PTv = PT[c]

        def yrow(mix):
            r0 = mix * 1024 + c * 128
            return slice(r0, r0 + 128)
        q = A.alloc([128, S], F32)
        kk = A.alloc([128, S], F32)
        tmp = A.alloc([128, S], F32)
        qn = A.alloc([128, S], BF16)
        kn = A.alloc([128, S], BF16)
        vtm = A.alloc([128, NCH, 128], BF16)
        vst = A.alloc([128, NCH, 128], F32)
        b_vst = P.buf("vst")
        ff = A.alloc([128, NCH], F32)
        lf = A.alloc([128, NCH], F32)
        cum = A.alloc([128, NCH], F32)
        car = A.alloc([128, NCH], F32)
        tot = A.alloc([128, NCH], F32)
        onesn = A.alloc([128, NCH], F32)
        bias = A.alloc([128, NCH, NCH], F32)
        pT = [A.alloc([128, 512], BF16) for _ in range(3)]
        rden = A.alloc([128, 512], F32)
        yo = A.alloc([128, S], BF16)
        rsq = A.alloc([128, 512], F32)
        (b_q, b_k, b_tmp, b_qn, b_kn, b_v, b_ff, b_lf, b_cum, b_car, b_tot, b_on, b_bias, b_rden, b_yo,
         b_rsq) = [P.buf(n) for n in ["q", "k", "tmp", "qn", "kn", "v", "ff", "lf", "cum", "car", "tot", "on",
                                       "bias", "rden", "yo", "rsq"]]
        b_pT = P.bufs(3, "pT")
        P.op("dve", lambda e: e.memset(onesn, 1.0), [], [b_on])

        def qknorm(src, dst_b, bsrc, bdst, gcol, scale):
            for qq in range(S // 512):
                tsl = slice(qq * 512, (qq + 1) * 512)
                tt("dve", tmp[:, tsl], src[:, tsl], src[:, tsl], ALU.mult, [bsrc], [b_tmp])
                pt, pb = nextps()
                mm(pt[:, :], ones_f, tmp[:, tsl], True, True, [b_cst, b_tmp], [pb])
                act(rsq, pt[:, :], AF.Sqrt, [pb, b_eps], [b_rsq], bias=eps_ap(1e-6), scale=1.0 / HD)
                recip(rsq, rsq, [b_rsq], [b_rsq])
                stt(tmp[:, tsl], src[:, tsl], foxp[:, gcol:gcol + 1], rsq, ALU.mult, ALU.mult, [bsrc, b_pp, b_rsq],
                    [b_tmp])
            ts("dve", dst_b, tmp, scale, None, ALU.mult, None, [b_tmp], [bdst])

        pi = 0
        for bl in range(BL):
            bs = slice(bl * S, (bl + 1) * S)
            P.dma("sp", q, PFv[6 * 128:7 * 128, bs], reads=[db["PF"]], writes=[b_q])
            P.dma("sp", kk, PFv[7 * 128:8 * 128, bs], reads=[db["PF"]], writes=[b_k])
            P.dma("sp", vst, PTv[bs, 256:384].rearrange("(n m) e -> m n e", m=128), reads=[db["PT"]], writes=[b_vst])
            cp("act", vtm, vst, [b_vst], [b_v])
            P.dma("sp", ff, PTf[c, :, bl * NCH:(bl + 1) * NCH], reads=[db["PTf"]], writes=[b_ff])
            qknorm(q, qn, b_q, b_qn, 0, float(HD ** -0.5))
            yield
            qknorm(kk, kn, b_k, b_kn, 1, 1.0)
            yield
            act(lf, ff, AF.Exp, [b_ff, b_nfb], [b_lf], bias=nfb, scale=-1.0)
            act(lf, lf, AF.Ln, [b_lf, b_eps], [b_lf], bias=eps_ap(1.0))
            ts("dve", lf, lf, -1.0, None, ALU.mult, None, [b_lf], [b_lf])
            pc, pcb = nextps()
            mm(pc[:, 0:NCH], triu_f, lf, True, True, [b_cst, b_lf], [pcb])
            mm(pc[:, 64:64 + NCH], ones_f, lf, True, True, [b_cst, b_lf], [pcb])
            cp("dve", tot, pc[:, 64:64 + NCH], [pcb], [b_tot])
            P.op("dve", lambda e: e.tensor_tensor_scan(out=car, data0=onesn, data1=tot, initial=0.0, op0=ALU.mult,
                                                       op1=ALU.add), [b_on, b_tot], [b_car])
            tt("dve", car, car, tot, ALU.subtract, [b_car, b_tot], [b_car])
            tt("dve", cum, pc[:, 0:NCH], car, ALU.add, [pcb, b_car], [b_cum])
            for qb in range(NCH):
                ts("dve", bias[:, qb, :], cum, -1.0, car[:, qb:qb + 1], ALU.mult, ALU.add, [b_cum, b_car], [b_bias])
            for sb in range(S // 512):
                po, pob = accps(2 * hi)
                pd, pdb = accps(2 * hi + 1)
                nkt = 4 * sb + 4
                for kt in range(nkt):
                    i0 = max(0, kt - 4 * sb)
                    q0 = sb * 512 + i0 * 128
                    w = 512 - i0 * 128
                    pS, pSb = nextps()
                    mm(pS[:, 0:w], kn[:, kt * 128:(kt + 1) * 128], qn[:, q0:q0 + w], True, True, [b_kn, b_qn], [pSb])
                    j = pi % 3
                    pi += 1
                    for i in range(i0, 4):
                        qb = 4 * sb + i
                        act(pT[j][:, (i - i0) * 128:(i - i0 + 1) * 128], pS[:, (i - i0) * 128:(i - i0 + 1) * 128],
                            AF.Exp, [pSb, b_bias], [b_pT[j]], bias=bias[:, qb, kt:kt + 1])
                    if kt >= 4 * sb:
                        tt("dve", pT[j][:, 0:128], pT[j][:, 0:128], triu_b, ALU.mult, [b_pT[j], b_cstb], [b_pT[j]])
                    mm(po[:, i0 * 128:512], vtm[:, kt, :], pT[j][:, 0:w], kt == 0, kt == nkt - 1, [b_v, b_pT[j]],
                       [pob])
                    mm(pd[:, i0 * 128:512], ones_b, pT[j][:, 0:w], kt == 0, kt == nkt - 1, [b_cstb, b_pT[j]], [pdb])
                    yield
                recip(rden, pd[:, :], [pdb], [b_rden])
                tt("dve", yo[:, sb * 512:(sb + 1) * 512], po[:, :], rden, ALU.mult, [pob, b_rden], [b_yo])
            P.dma("sp", yv[yrow(3), bs], yo, reads=[b_yo], writes=[db["yT"]])
            yield

    def phase_mixers_all(l):
        m0 = A.mark()
        rot = A.alloc([128, 2, S], F32)
        b_rot = P.buf("rot")
        P.dma("sp", rot, I["rot"].rearrange("p (t s) -> p t s", t=2), writes=[b_rot])
        for c0 in range(0, 8, 2):
            mA = A.mark()
            hps = [load_head(l, c0 + i, rot, b_rot) for i in range(2)]
            for gi, gen in enumerate((g_sgu, g_lru, g_ret, g_fox)):
                m1 = A.mark()
                psmode[0] = 4 if gen is g_fox else 6
                psi[0] = 0
                wcast_slice(4 + (c0 // 2) * 4 + gi, 20)
                run_gens([gen(l, c0 + i, hps[i], i) for i in range(2)])
                P.barrier()
                psmode[0] = 6
                A.release(m1)
            A.release(mA)
        A.release(m0)

    def phase_outproj(l, src_ap, src_buf):
        m0 = A.mark()
        FG = min(8, KC)
        wo = A.alloc([128, 32, FG * 128], BF16)
        b_wo = P.buf("wo")
        yb = [A.alloc([128, 32, 512], BF16) for _ in range(2)]
        b_yb = P.bufs(2, "yb")
        xk = [A.alloc([128, 512], F32) for _ in range(3)]
        b_xk = P.bufs(3, "xk")
        yvv = yT.ap().rearrange("(k p) t -> p k t", p=128)
        wv = I["wout"][l].rearrange("(k p) d -> p k d", p=128)
        sv = src_ap.rearrange("(k p) t -> p k t", p=128)
        dv = xres.ap().rearrange("(k p) t -> p k t", p=128)
        ci = 0
        li = 0
        for fg in range(KC // FG):
            for k8 in range(0, 32, 8):
                P.dma("pool", wo[:, k8:k8 + 8, :], wv[:, k8:k8 + 8, fg * FG * 128:(fg + 1) * FG * 128], writes=[b_wo])
            for tb in range(NBK):
                bl = tb // BPB
                tsl = slice(tb * 512, (tb + 1) * 512)
                u = li % 2
                li += 1
                P.dma("sp", yb[u], yvv[:, :, tsl], reads=[db["yT"]], writes=[b_yb[u]])
                for f8 in range(FG):
                    fo = fg * FG + f8
                    i = ci % 3
                    ci += 1
                    P.dma("sp", xk[i], sv[:, fo, tsl], reads=[src_buf], writes=[b_xk[i]])
                    pt, pb = nextps()
                    for kq in range(32):
                        mm(pt[:, :], wo[:, kq, f8 * 128:(f8 + 1) * 128], yb[u][:, kq, :], kq == 0, kq == 31,
                           [b_wo, b_yb[u]], [pb])
                    stt(xk[i], pt[:, :], MV(bl, l, 2)[:, fo:fo + 1], xk[i], ALU.mult, ALU.add,
                        [pb, b_xk[i], b_modv], [b_xk[i]])
                    P.dma("sp", dv[:, fo, tsl], xk[i], reads=[b_xk[i]], writes=[db["xres"]])
        P.barrier()
        A.release(m0)

    def make_moe_extra(l):
        st = {}

        def extra(kind, args):
            if kind == "alloc":
                st["rwb"] = A.alloc([128, KC, E], BF16)
                st["rb"] = A.alloc([128, E], F32)
                st["b_rw"] = P.buf("rw")
                P.dma("pool", st["rwb"], I["rw"][l].rearrange("(k p) e -> p k e", p=128), writes=[st["b_rw"]])
                P.dma("sp", st["rb"], I["rbbc"][:, l * E:(l + 1) * E], writes=[st["b_rw"]])
                st["sc"] = A.alloc([128, E], F32)
                st["sel"] = A.alloc([128, E], F32)
                st["top"] = A.alloc([128, 8], F32)
                st["den"] = A.alloc([128, 1], F32)
                st["gts"] = A.alloc([128, 4, E], F32)
                st["gT"] = A.alloc([E, 512], F32)
                for n in ["sc", "sel", "top", "den", "gts", "gT"]:
                    st["b_" + n] = P.buf(n)
                st["swg"] = A.alloc([128, KC, 256], BF16)
                st["swu"] = A.alloc([128, KC, 256], BF16)
                st["swd"] = A.alloc([128, 2, D], BF16)
                st["b_sw"] = P.buf("sw")
                P.dma("pool", st["swg"], I["swg"][l].rearrange("(k p) f -> p k f", p=128), writes=[st["b_sw"]])
                P.dma("pool", st["swu"], I["swu"][l].rearrange("(k p) f -> p k f", p=128), writes=[st["b_sw"]])
                P.dma("pool", st["swd"], I["swd"][l].rearrange("(f p) d -> p f d", p=128), writes=[st["b_sw"]])
                st["sg"] = A.alloc([128, 512], F32)
                st["hid"] = A.alloc([128, 2, 512], BF16)
                st["b_sg"], st["b_hid"] = P.buf("sg"), P.buf("hid")
                st["xs"] = [A.alloc([128, 512], F32) for _ in range(2)]
                st["b_xs"] = P.bufs(2, "xs")
                return st
            if kind == "post":
                tb, bl, hb, b_h = args
                for t4 in range(4):
                    pt, pb = nextps()
                    for kc in range(KC):
                        mm(pt[:, 0:E], hb[:, kc, t4 * 128:(t4 + 1) * 128], st["rwb"][:, kc, :], kc == 0, kc == KC - 1,
                           [b_h, st["b_rw"]], [pb])
                    act(st["sc"], pt[:, 0:E], AF.Sigmoid, [pb], [st["b_sc"]])
                    tt("dve", st["sel"], st["sc"], st["rb"], ALU.add, [st["b_sc"], st["b_rw"]], [st["b_sel"]])
                    P.op("dve", lambda e: e.max(out=st["top"], in_=st["sel"]), [st["b_sel"]], [st["b_top"]])
                    ts("dve", st["sel"], st["sel"], st["top"][:, 7:8], None, ALU.is_ge, None,
                       [st["b_sel"], st["b_top"]], [st["b_sel"]])
                    tt("dve", st["sel"], st["sel"], st["sc"], ALU.mult, [st["b_sel"], st["b_sc"]], [st["b_sel"]])
                    P.op("dve", lambda e: e.tensor_reduce(out=st["den"], in_=st["sel"], axis=AX.X, op=ALU.add),
                         [st["b_sel"]], [st["b_den"]])
                    recip(st["den"], st["den"], [st["b_den"]], [st["b_den"]])
                    ts("dve", st["gts"][:, t4, :], st["sel"], st["den"][:, 0:1], 2.5, ALU.mult, ALU.mult,
                       [st["b_sel"], st["b_den"]], [st["b_gts"]])
                pt, pb = nextps()
                for t4 in range(4):
                    tr(pt[0:E, t4 * 128:(t4 + 1) * 128], st["gts"][:, t4, :], ident_f, [st["b_gts"], b_cst], [pb])
                cp("dve", st["gT"], pt[0:E, :], [pb], [st["b_gT"]])
                P.dma("sp", gTd.ap()[:, tb * 512:(tb + 1) * 512], st["gT"], reads=[st["b_gT"]], writes=[db["gTd"]])
                for f in range(2):
                    pg, pgb = nextps()
                    pu, pub = nextps()
                    for kc in range(KC):
                        mm(pg[:, :], st["swg"][:, kc, f * 128:(f + 1) * 128], hb[:, kc, :], kc == 0, kc == KC - 1,
                           [st["b_sw"], b_h], [pgb])
                    for kc in range(KC):
                        mm(pu[:, :], st["swu"][:, kc, f * 128:(f + 1) * 128], hb[:, kc, :], kc == 0, kc == KC - 1,
                           [st["b_sw"], b_h], [pub])
                    act(st["sg"], pg[:, :], AF.Silu, [pgb], [st["b_sg"]])
                    tt("dve", st["hid"][:, f, :], st["sg"], pu[:, :], ALU.mult, [st["b_sg"], pub], [st["b_hid"]])
                xs = st["xs"]
                for fo in range(KC):
                    po, pob = nextps()
                    for f in range(2):
                        mm(po[:, :], st["swd"][:, f, fo * 128:(fo + 1) * 128], st["hid"][:, f, :], f == 0, f == 1,
                           [st["b_sw"], st["b_hid"]], [pob])
                    i = fo % 2
                    ts("dve", xs[i], po[:, :], MV(bl, l, 5)[:, fo:fo + 1], None, ALU.mult, None,
                       [pob, b_modv], [st["b_xs"][i]])
                    P.dma("sp", shr.ap()[fo * 128:(fo + 1) * 128, tb * 512:(tb + 1) * 512], xs[i],
                          reads=[st["b_xs"][i]], writes=[db["shr"]])
                return None
        return extra

    def wcast_jobs(l):
        jobs = []
        for e in range(E):
            jobs.append(lambda e=e: P.dma("pool", wgb.ap()[e], I["ewg"][l, e], writes=[db["wgb"]], bg=True))
            jobs.append(lambda e=e: P.dma("pool", wub.ap()[e], I["ewu"][l, e], writes=[db["wub"]], bg=True))
        for g in range(NG):
            src = I["ewd"][l, g * 8:(g + 1) * 8].rearrange("j (f p) (dg c) -> dg p j f c", p=128, c=256)
            dst = wdb.ap()[g].rearrange("dg p (j f c) -> dg p j f c", j=8, f=2)
            for dg in range(DG):
                jobs.append(lambda src=src, dst=dst, dg=dg: P.dma("pool", dst[dg], src[dg], writes=[db["wdb"]],
                                                                  bg=True))
        return jobs

    wjobs = []

    def wcast_slice(k, n):
        tot = len(wjobs)
        for j in range(k * tot // n, (k + 1) * tot // n):
            wjobs[j]()

    def phase_moe(l, dst_ap, dst_buf):
        m0 = A.mark()
        acc = A.alloc([128, KC, 512], F32)
        hbk = A.alloc([128, KC, 512], BF16)
        hid = A.alloc([128, 16, 512], BF16)
        gsel = A.alloc([E, 8 * 128], F32)
        gblk = A.alloc([E, 512], F32)
        sg = [A.alloc([128, 512], F32) for _ in range(2)]
        wd = [A.alloc([128, 16, 256], BF16) for _ in range(2)]
        wg = [A.alloc([128, KC, 128], BF16) for _ in range(2)]
        wu = [A.alloc([128, KC, 128], BF16) for _ in range(2)]
        KG = 2
        x1k = [A.alloc([128, KG, 512], F32) for _ in range(2)]
        shk = [A.alloc([128, KG, 512], F32) for _ in range(2)]
        b_acc, b_hbk, b_hid, b_gsel, b_gblk = [P.buf(n) for n in ["acc", "hbk", "hid", "gsel", "gblk"]]
        b_sg = P.bufs(2, "sg")
        b_wd = P.bufs(2, "wd")
        b_wg = P.bufs(2, "wg")
        b_wu = P.bufs(2, "wu")
        b_x1k, b_shk = P.bufs(2, "x1k"), P.bufs(2, "shk")
        wi = 0
        di = 0
        it = 0
        hv = hT.ap().rearrange("(k p) t -> p k t", p=128)
        xv = xres.ap().rearrange("(k p) t -> p k t", p=128)
        sv = shr.ap().rearrange("(k p) t -> p k t", p=128)
        dv = dst_ap.rearrange("(k p) t -> p k t", p=128)
        wgv = wgb.ap().rearrange("e f p (k c) -> e f p k c", c=128)
        wuv = wub.ap().rearrange("e f p (k c) -> e f p k c", c=128)
        wdv = wdb.ap().rearrange("g dg p (q c) -> g dg p q c", c=256)
        for tb in range(NBK):
            bl = tb // BPB
            tsl = slice(tb * 512, (tb + 1) * 512)
            P.dma("sp", hbk, hv[:, :, tsl], reads=[db["hT"]], writes=[b_hbk])
            P.dma("sp", gblk, gTd.ap()[:, tsl], reads=[db["gTd"]], writes=[b_gblk])
            for g in range(NG):
                P.dma("sp", gsel, I["selB"][g], writes=[b_gsel])
                for j in range(8):
                    e = g * 8 + j
                    for f in range(2):
                        w = wi % 2
                        wi += 1
                        P.dma("sp", wg[w], wgv[e, f], reads=[db["wgb"]], writes=[b_wg[w]])
                        P.dma("sp", wu[w], wuv[e, f], reads=[db["wub"]], writes=[b_wu[w]])
                        pg, pgb = nextps()
                        pu, pub = nextps()
                        for kc in range(KC):
                            mm(pg[:, :], wg[w][:, kc, :], hbk[:, kc, :], kc == 0, kc == KC - 1, [b_wg[w], b_hbk], [pgb])
                        for kc in range(KC):
                            mm(pu[:, :], wu[w][:, kc, :], hbk[:, kc, :], kc == 0, kc == KC - 1, [b_wu[w], b_hbk], [pub])
                        pb_, pbb = nextps()
                        mm(pb_[:, :], gsel[:, j * 128:(j + 1) * 128], gblk, True, True, [b_gsel, b_gblk], [pbb])
                        s = wi % 2
                        act(sg[s], pg[:, :], AF.Silu, [pgb], [b_sg[s]])
                        tt("dve", sg[s], sg[s], pu[:, :], ALU.mult, [b_sg[s], pub], [b_sg[s]])
                        tt("dve", hid[:, 2 * j + f, :], sg[s], pb_[:, :], ALU.mult, [b_sg[s], pbb], [b_hid])
                for dg in range(DG):
                    w = di % 2
                    di += 1
                    P.dma("sp", wd[w], wdv[g, dg], reads=[db["wdb"]], writes=[b_wd[w]])
                    for f2 in range(2):
                        fo = dg * 2 + f2
                        po, pob = nextps()
                        for kq in range(16):
                            mm(po[:, :], wd[w][:, kq, f2 * 128:(f2 + 1) * 128], hid[:, kq, :], kq == 0, kq == 15,
                               [b_wd[w], b_hid], [pob])
                        if g == 0:
                            cp("act", acc[:, fo, :], po[:, :], [pob], [b_acc])
                        else:
                            tt("dve", acc[:, fo, :], acc[:, fo, :], po[:, :], ALU.add, [b_acc, pob], [b_acc])
            for k0 in range(0, KC, KG):
                i = it % 2
                it += 1
                ksl = slice(k0, k0 + KG)
                P.dma("sp", x1k[i], xv[:, ksl, tsl], reads=[db["xres"]], writes=[b_x1k[i]])
                P.dma("sp", shk[i], sv[:, ksl, tsl], reads=[db["shr"]], writes=[b_shk[i]])
                tt("dve", x1k[i], x1k[i], shk[i], ALU.add, [b_x1k[i], b_shk[i]], [b_x1k[i]])
                for kk_ in range(KG):
                    kc = k0 + kk_
                    stt(x1k[i][:, kk_, :], acc[:, kc, :], MV(bl, l, 5)[:, kc:kc + 1], x1k[i][:, kk_, :],
                        ALU.mult, ALU.add, [b_acc, b_x1k[i], b_modv], [b_x1k[i]])
                P.dma("sp", dv[:, ksl, tsl], x1k[i], reads=[b_x1k[i]], writes=[dst_buf])
        P.barrier()
        A.release(m0)

    phase_mod()
    cur_ap, cur_buf = I["xT"], NB
    for l in range(L):
        phase_norm(l, 0, cur_ap, cur_buf)
        phase_inproj_all(l)
        wjobs[:] = wcast_jobs(l)
        phase_sgu_stats(l)
        phase_mixers_all(l)
        phase_outproj(l, cur_ap, cur_buf)
        phase_norm(l, 1, xres.ap(), db["xres"], extra=make_moe_extra(l))
        last = (l == L - 1)
        phase_moe(l, outT if last else xres.ap(), db["outT"] if last else db["xres"])
        cur_ap, cur_buf = xres.ap(), db["xres"]
    P.barrier(final=True)
    P.emit()
    print("prog stats", P.stats, flush=True)
    es.close()
    return nc


def _fm(v, kc):
    return np.ascontiguousarray(np.asarray(v, np.float32).reshape(kc, 128).T)


def host_consts(cfg):
    S = cfg.S
    ident = np.eye(128, dtype=np.float32)
    idx = np.arange(128)
    triu = (idx[:, None] <= idx[None, :]).astype(np.float32)
    ones = np.ones((128, 128), np.float32)
    Pm = np.zeros((128, 128), np.float32)
    for d in range(64):
        Pm[d + 64, d] = -1.0
        Pm[d, d + 64] = 1.0
    cst = np.concatenate([ident, triu, ones, Pm], axis=1).astype(np.float32)
    hc = []
    for c in range(8):
        lg = np.log1p(-2.0 ** (-5.0 - c))
        rel = (idx[None, :] - idx[:, None]).astype(np.float64)
        decT = np.where(rel >= 0, np.exp(rel * lg), 0.0).astype(np.float32)
        qd = np.exp((idx + 1) * lg).astype(np.float32)
        qdbc = np.tile(qd[None, :], (128, 1))
        kd = np.exp((127 - idx) * lg).astype(np.float32)[:, None]
        cd = np.full((128, 1), np.exp(128 * lg), np.float32)
        hc.append(np.concatenate([decT, qdbc, kd, cd], axis=1))
    hcst = np.stack(hc).astype(np.float32)
    half = 64
    inv = (10000.0 ** (-np.arange(half, dtype=np.float32) / half)).astype(np.float32)
    pos = np.arange(S, dtype=np.float32)
    ang = (pos[:, None] * inv[None, :]).astype(np.float32)
    cosT = np.cos(ang).T.astype(np.float32)
    sinT = np.sin(ang).T.astype(np.float32)
    rot = np.concatenate([np.concatenate([cosT, cosT], 0), np.concatenate([sinT, sinT], 0)], axis=1)
    return cst, hcst, np.ascontiguousarray(rot.astype(np.float32))


def make_in_maps(cfg, inp):
    D, S, B, E, L, KC, BL, NTL, NG = cfg.D, cfg.S, cfg.B, cfg.E, cfg.L, cfg.KC, cfg.BL, cfg.NTL, cfg.NG
    f32 = np.float32
    A_ = lambda k: np.asarray(inp[k], f32)
    x = A_("x")
    cvec = A_("c")
    sh = {}
    sh["cT"] = np.ascontiguousarray(cvec.T.reshape(KC, 128, B).transpose(1, 0, 2).reshape(128, KC * B))
    sh["wada"] = A_("w_ada")
    sh["bada"] = _fm(A_("b_ada"), 6 * KC)
    sh["tab"] = np.concatenate([_fm(A_("ada_table")[l].reshape(-1), 6 * KC) for l in range(L)], 1)
    sh["ng"] = np.concatenate([np.concatenate([_fm(A_("norm1_g")[l], KC), _fm(A_("norm2_g")[l], KC)], 1)
                               for l in range(L)], 1)
    w_in = A_("w_in")
    order = [0, 1, 3, 4, 5, 6, 8, 9, 2, 7, 10]
    win = np.empty((L, 8, D, 1409), f32)
    for c in range(8):
        for i, blk in enumerate(order):
            win[:, c, :, i * 128:(i + 1) * 128] = w_in[:, :, blk * 1024 + c * 128: blk * 1024 + (c + 1) * 128]
        win[:, c, :, 1408] = w_in[:, :, 11 * 1024 + c]
    sh["win"] = win
    sh["wout"] = A_("w_out")
    lrup = np.empty((128, L, 8, 8), f32)
    sgbc = np.empty((128, L, 8, 2, 128), f32)
    foxp = np.empty((128, L, 8, 3), f32)
    for l in range(L):
        for c in range(8):
            sl = slice(c * 128, (c + 1) * 128)
            for j in range(4):
                lrup[:, l, c, j] = A_("lru_conv_w")[l, j, sl]
            lrup[:, l, c, 4] = A_("lru_conv_b")[l, sl]
            lrup[:, l, c, 5] = A_("lru_ba")[l, sl]
            lrup[:, l, c, 6] = A_("lru_bx")[l, sl]
            lrup[:, l, c, 7] = A_("lru_lambda")[l, sl]
            sgbc[:, l, c, 0, :] = A_("sgu_norm_g")[l, sl][None, :]
            sgbc[:, l, c, 1, :] = A_("sgu_b")[l, c][None, :]
            foxp[:, l, c, 0] = A_("fox_qn")[l]
            foxp[:, l, c, 1] = A_("fox_kn")[l]
            foxp[:, l, c, 2] = A_("fox_fb")[l, c]
    sh["lrup"] = lrup.reshape(128, -1)
    sh["sgbc"] = sgbc.reshape(128, -1)
    sh["foxp"] = foxp.reshape(128, -1)
    sh["lruw"] = np.ascontiguousarray(np.stack([A_("lru_wa"), A_("lru_wx")], axis=2))
    sh["sgwT"] = np.ascontiguousarray(A_("sgu_w").transpose(0, 1, 3, 2))
    sh["rw"] = A_("router_w")
    sh["rbbc"] = np.ascontiguousarray(np.concatenate([np.tile(A_("router_bias")[l][None, :], (128, 1))
                                                      for l in range(L)], 1))

    def relay(w):
        w = w.reshape(L, E, KC, 128, 2, 128).transpose(0, 1, 4, 3, 2, 5)
        return np.ascontiguousarray(w.reshape(L, E, 2, 128, KC * 128))
    sh["ewg"] = relay(A_("exp_w_gate"))
    sh["ewu"] = relay(A_("exp_w_up"))
    sh["ewd"] = A_("exp_w_down")
    sh["swg"] = A_("sh_w_gate")
    sh["swu"] = A_("sh_w_up")
    sh["swd"] = A_("sh_w_down")
    selB = np.zeros((NG, E, 8 * 128), f32)
    for g in range(NG):
        for j in range(8):
            selB[g, g * 8 + j, j * 128:(j + 1) * 128] = 1.0
    sh["selB"] = selB
    sh["cst"], sh["hcst"], sh["rot"] = host_consts(cfg)
    sh = {k: np.ascontiguousarray(v, dtype=f32) for k, v in sh.items()}
    maps = []
    for c in range(cfg.NCORE):
        m = dict(sh)
        xb = x[c * BL:(c + 1) * BL].reshape(NTL, D)
        m["xT"] = np.ascontiguousarray(xb.T)
        sel = np.zeros((128, BL, B), f32)
        for bl in range(BL):
            sel[:, bl, c * BL + bl] = 1.0
        m["selb"] = sel.reshape(128, BL * B)
        maps.append(m)
    return maps


_NC_CACHE = {}


def run(cfg, inputs):
    key = (cfg.D, cfg.S, cfg.B, cfg.E, cfg.L, cfg.NCORE)
    if key not in _NC_CACHE:
        _NC_CACHE[key] = build(cfg)
    nc = _NC_CACHE[key]
    maps = make_in_maps(cfg, inputs)
    res = run_bass_kernel_spmd(nc, maps, core_ids=list(range(cfg.NCORE)))
    outs = [r["outT"] for r in res.results]
    full = np.concatenate([o.T for o in outs], axis=0)
    return np.ascontiguousarray(full.reshape(cfg.B, cfg.S, cfg.D).astype(np.float32))


def kernel(**inputs):
    return run(Cfg(), inputs)
```
